# Optimizing a Trainium2 kernel written in Bass

```python
import math
import jax, jax.numpy as jnp
from jax import lax
import numpy as np

D_MODEL = 1024
BATCH = 16
SEQ = 2048
DEPTH = 1

D_HY = D_MODEL
HY_ORDER = 2
HY_SHORT_CONV = 3
HY_EMB_DIM = 33
HY_FILTER_HIDDEN = 64
HY_FAST_DECAY_PCT = 0.3
HY_SLOW_DECAY_PCT = 1.5
HY_DECAY_TARGET = 1e-2
HY_FILTER_OUT_STD = 0.01
HY_COLS = (HY_ORDER + 1) * D_HY

SSD_EXPAND = 2
D_SSM = SSD_EXPAND * D_MODEL
SSD_HEADDIM = 64
SSD_HEADS = D_SSM // SSD_HEADDIM
SSD_GROUPS = 8
SSD_STATE = 128
SSD_CONV = 5
SSD_CHUNK = 128
SSD_CONV_DIM = D_SSM + 2 * SSD_GROUPS * SSD_STATE
SSD_DT_MIN = 1e-3
SSD_DT_MAX = 1e-1
SSD_COLS = D_SSM + SSD_CONV_DIM + 2 * SSD_HEADS

N_BRANCH = 2
GATE_COLS = N_BRANCH * D_MODEL
IN_COLS = HY_COLS + SSD_COLS + GATE_COLS

N_EXPERTS = 64
TOP_K = 8
N_EXPERT_GROUPS = 8
TOPK_GROUPS = 4
D_EXPERT = 256
D_SHARED = 256
ROUTED_SCALE = 2.5
MOE_BLOCK = 128

DN_ALPHA = (2.0 * DEPTH) ** 0.25
DN_BETA = (8.0 * DEPTH) ** -0.25
LN_EPS = 1e-5
RMS_EPS = 1e-5

kernel_name = "hyena_ssd_gated_moe_deepnorm_block"


def layer_norm(x, g, b):
    xf = x.astype(jnp.float32)
    mu = jnp.mean(xf, axis=-1, keepdims=True)
    var = jnp.mean(jnp.square(xf - mu), axis=-1, keepdims=True)
    return ((xf - mu) * lax.rsqrt(var + LN_EPS) * g + b).astype(x.dtype)


def centred_dwconv(x, w, b):
    k_w = w.shape[0]
    seq = x.shape[1]
    pad = k_w // 2
    xp = jnp.pad(x, ((0, 0), (pad, pad), (0, 0)))
    y = b
    for k in range(k_w):
        y = y + xp[:, k:k + seq] * w[k]
    return y


def hyena_pos_features(seq):
    t = jnp.linspace(0.0, 1.0, seq, dtype=jnp.float32)[:, None]
    bands = (HY_EMB_DIM - 1) // 2
    w = 2.0 * math.pi * jnp.arange(seq, dtype=jnp.float32)[:, None] / seq
    f = jnp.linspace(1e-4, bands - 1, bands, dtype=jnp.float32)[None, :]
    return jnp.concatenate([t, jnp.cos(f * w), -jnp.sin(f * w)], axis=-1)


def hyena_decay(seq):
    t = jnp.linspace(0.0, 1.0, seq, dtype=jnp.float32)[:, None]
    max_decay = math.log(HY_DECAY_TARGET) / HY_FAST_DECAY_PCT
    min_decay = math.log(HY_DECAY_TARGET) / HY_SLOW_DECAY_PCT
    deltas = jnp.linspace(min_decay, max_decay, D_HY, dtype=jnp.float32)[None, :]
    return jnp.exp(-t * jnp.abs(deltas))


def hyena_filters(seq, w1, b1, fr1, w2, b2, fr2, w3, b3, fr3, w4):
    f32 = jnp.float32
    z = hyena_pos_features(seq)
    h = jnp.sin(fr1.astype(f32) * (z @ w1.astype(f32) + b1.astype(f32)))
    h = jnp.sin(fr2.astype(f32) * (h @ w2.astype(f32) + b2.astype(f32)))
    h = jnp.sin(fr3.astype(f32) * (h @ w3.astype(f32) + b3.astype(f32)))
    k = (h @ w4.astype(f32)).reshape(seq, HY_ORDER, 2, D_HY) * hyena_decay(seq)[:, None, None, :]
    kf, kb = k[:, :, 0], k[:, :, 1]
    k_full = jnp.concatenate([kf[:1] + kb[:1], kf[1:], jnp.zeros_like(kf[:1]), kb[:0:-1]], axis=0)
    return jnp.fft.rfft(k_full, axis=0)


def fft_long_conv(u, k_spec, bias):
    seq = u.shape[1]
    uf32 = u.astype(jnp.float32)
    uf = jnp.fft.rfft(uf32, n=2 * seq, axis=1)
    y = jnp.fft.irfft(uf * k_spec[None], n=2 * seq, axis=1)[:, :seq]
    return (y + uf32 * bias.astype(jnp.float32)).astype(u.dtype)


def hyena_mixer(p_hy, conv_w, conv_b, k_spec, hy_bias):
    hy = centred_dwconv(p_hy, conv_w, conv_b)
    g1, g2, v = jnp.split(hy, HY_ORDER + 1, axis=-1)
    z = g1 * fft_long_conv(v, k_spec[:, 0], hy_bias[0])
    return g2 * fft_long_conv(z, k_spec[:, 1], hy_bias[1])


def ssd_chunked(xdt, a, bm, cm):
    b, l, h, p = xdt.shape
    g, n = bm.shape[-2:]
    r = h // g
    c = l // SSD_CHUNK
    q = SSD_CHUNK
    X = xdt.reshape(b, c, q, g, r, p)
    A_cs = jnp.cumsum(a.reshape(b, c, q, g, r), axis=2)
    Bc = bm.reshape(b, c, q, g, n)
    Cc = cm.reshape(b, c, q, g, n)
    seg = A_cs[:, :, :, None] - A_cs[:, :, None, :]
    tril = jnp.tril(jnp.ones((q, q), dtype=bool))[:, :, None, None]
    decay = jnp.exp(jnp.where(tril, seg, -jnp.inf))
    CB = jnp.einsum("bclgn,bcsgn->bclsg", Cc, Bc)
    y_diag = jnp.einsum("bclsg,bclsgr,bcsgrp->bclgrp", CB, decay, X)
    to_end = jnp.exp(A_cs[:, :, -1:] - A_cs)
    states = jnp.einsum("bcsgn,bcsgr,bcsgrp->bcgrpn", Bc, to_end, X)
    chunk_decay = jnp.exp(A_cs[:, :, -1])

    def step(carry, inp):
        st, dec = inp
        return carry * dec[..., None, None] + st, carry

    init = jnp.zeros((b, g, r, p, n), states.dtype)
    _, prev = lax.scan(step, init, (jnp.moveaxis(states, 1, 0), jnp.moveaxis(chunk_decay, 1, 0)))
    prev = jnp.moveaxis(prev, 0, 1)
    y_off = jnp.einsum("bclgn,bcgrpn,bclgr->bclgrp", Cc, prev, jnp.exp(A_cs))
    return (y_diag + y_off).reshape(b, l, h, p)


def ssd_mixer(p_z, p_xbc, p_dt, conv_w, conv_b, dt_bias, a_log, d_skip, norm_w):
    f32 = jnp.float32
    bsz, seq, _ = p_z.shape
    xbc = jax.nn.silu(centred_dwconv(p_xbc, conv_w, conv_b))
    xs, bm, cm = jnp.split(xbc, [D_SSM, D_SSM + SSD_GROUPS * SSD_STATE], axis=-1)
    xs = xs.reshape(bsz, seq, SSD_HEADS, SSD_HEADDIM)
    bm = bm.reshape(bsz, seq, SSD_GROUPS, SSD_STATE)
    cm = cm.reshape(bsz, seq, SSD_GROUPS, SSD_STATE)
    dt = jax.nn.softplus(p_dt.astype(f32).reshape(bsz, seq, 2, SSD_HEADS) + dt_bias.astype(f32))
    a = dt * -jnp.exp(a_log.astype(f32))
    flip = lambda t: jnp.flip(t, axis=1)
    y_fwd = ssd_chunked(xs * dt[:, :, 0, :, None], a[:, :, 0], bm, cm)
    y_bwd = flip(ssd_chunked(flip(xs * dt[:, :, 1, :, None]), flip(a[:, :, 1]), flip(bm), flip(cm)))
    y = y_fwd + y_bwd + xs * d_skip[:, None]
    y = y.reshape(bsz, seq, D_SSM) * jax.nn.silu(p_z)
    yg = y.astype(f32).reshape(bsz, seq, SSD_GROUPS, D_SSM // SSD_GROUPS)
    yg = yg * lax.rsqrt(jnp.mean(jnp.square(yg), axis=-1, keepdims=True) + RMS_EPS)
    return (yg.reshape(bsz, seq, D_SSM) * norm_w).astype(p_z.dtype)


def mixer_sublayer(u, w_in, b_in, hy_conv_w, hy_conv_b, hy_f_w1, hy_f_b1, hy_f_freq1,
                   hy_f_w2, hy_f_b2, hy_f_freq2, hy_f_w3, hy_f_b3, hy_f_freq3, hy_f_w4, hy_bias,
                   ssd_conv_w, ssd_conv_b, ssd_dt_bias, ssd_a_log, ssd_d, ssd_norm_w,
                   w_hy_branch, w_ssd_branch, w_out, b_out):
    proj = u @ w_in + b_in
    splits = np.cumsum([HY_COLS, D_SSM, SSD_CONV_DIM, 2 * SSD_HEADS]).tolist()
    p_hy, p_z, p_xbc, p_dt, p_gate = jnp.split(proj, splits, axis=-1)
    k_spec = hyena_filters(u.shape[1], hy_f_w1, hy_f_b1, hy_f_freq1, hy_f_w2, hy_f_b2, hy_f_freq2,
                           hy_f_w3, hy_f_b3, hy_f_freq3, hy_f_w4)
    y_hy = hyena_mixer(p_hy, hy_conv_w, hy_conv_b, k_spec, hy_bias) @ w_hy_branch
    y_ssd = ssd_mixer(p_z, p_xbc, p_dt, ssd_conv_w, ssd_conv_b, ssd_dt_bias, ssd_a_log,
                      ssd_d, ssd_norm_w) @ w_ssd_branch
    g_hy, g_ssd = jnp.split(jax.nn.sigmoid(p_gate), N_BRANCH, axis=-1)
    return (g_hy * y_hy + g_ssd * y_ssd) @ w_out + b_out


def route(h2d, router_w, router_bias):
    n_tok = h2d.shape[0]
    scores = jax.nn.sigmoid((h2d @ router_w).astype(jnp.float32))
    biased = scores + router_bias.astype(jnp.float32)
    per_group = N_EXPERTS // N_EXPERT_GROUPS
    grp_score = lax.top_k(biased.reshape(n_tok, N_EXPERT_GROUPS, per_group), 2)[0].sum(-1)
    _, top_grp = lax.top_k(grp_score, TOPK_GROUPS)
    grp_mask = jnp.any(top_grp[..., None] == jnp.arange(N_EXPERT_GROUPS), axis=-2)
    masked = jnp.where(jnp.repeat(grp_mask, per_group, axis=-1), biased, -jnp.inf)
    _, idx = lax.top_k(masked, TOP_K)
    w = jnp.take_along_axis(scores, idx, axis=-1)
    w = w / jnp.sum(w, axis=-1, keepdims=True) * ROUTED_SCALE
    return idx, w


def routed_experts(h2d, idx, w, w_gate, w_up, w_down):
    n_tok, d = h2d.shape
    n_assign = n_tok * TOP_K
    flat_e = idx.reshape(n_assign)
    flat_tok = jnp.repeat(jnp.arange(n_tok, dtype=jnp.int32), TOP_K)
    flat_w = w.reshape(n_assign)
    order = jnp.argsort(flat_e)
    e_sorted = flat_e[order]
    counts = jnp.bincount(flat_e, length=N_EXPERTS)
    padded = (counts + MOE_BLOCK - 1) // MOE_BLOCK * MOE_BLOCK
    pad_end = jnp.cumsum(padded)
    pad_start = pad_end - padded
    sorted_start = jnp.cumsum(counts) - counts
    dest = pad_start[e_sorted] + jnp.arange(n_assign) - sorted_start[e_sorted]
    n_blocks = -(-n_assign // MOE_BLOCK) + N_EXPERTS
    n_slots = n_blocks * MOE_BLOCK
    slot_tok = jnp.full((n_slots,), n_tok, jnp.int32).at[dest].set(flat_tok[order])
    slot_w = jnp.zeros((n_slots,), jnp.float32).at[dest].set(flat_w[order])
    block_e = jnp.minimum(jnp.searchsorted(pad_end, jnp.arange(n_blocks) * MOE_BLOCK, side="right"),
                          N_EXPERTS - 1)
    h_pad = jnp.concatenate([h2d, jnp.zeros((1, d), h2d.dtype)], axis=0)

    def step(acc, blk):
        tok, wt, e = blk
        xb = h_pad[tok]
        hb = jax.nn.silu(xb @ w_gate[e]) * (xb @ w_up[e])
        yb = (hb @ w_down[e]) * wt[:, None]
        return acc.at[tok].add(yb.astype(acc.dtype)), None

    acc, _ = lax.scan(step, jnp.zeros_like(h_pad),
                      (slot_tok.reshape(n_blocks, MOE_BLOCK), slot_w.reshape(n_blocks, MOE_BLOCK), block_e))
    return acc[:n_tok]


def moe_sublayer(h, router_w, router_bias, exp_w_gate, exp_w_up, exp_w_down,
                 sh_w_gate, sh_w_up, sh_w_down):
    bsz, seq, d = h.shape
    h2d = h.reshape(bsz * seq, d)
    idx, w = route(h2d, router_w, router_bias)
    routed = routed_experts(h2d, idx, w, exp_w_gate, exp_w_up, exp_w_down)
    shared = (jax.nn.silu(h2d @ sh_w_gate) * (h2d @ sh_w_up)) @ sh_w_down
    return (routed + shared).reshape(bsz, seq, d)


def setup_inputs(seed: int = 0) -> dict:
    key = jax.random.key(seed)
    ks = iter(jax.random.split(key, 64))
    f32 = jnp.float32
    Ld = DEPTH

    def nrm(shape, std):
        return std * jax.random.normal(next(ks), shape, f32)

    def xavier(fan_in, fan_out, shape, gain=1.0):
        return nrm(shape, gain * math.sqrt(2.0 / (fan_in + fan_out)))

    def ones_noise(shape):
        return 1.0 + nrm(shape, 0.01)

    u_dt = jax.random.uniform(next(ks), (Ld, 2, SSD_HEADS), f32)
    dt0 = jnp.exp(u_dt * (math.log(SSD_DT_MAX) - math.log(SSD_DT_MIN)) + math.log(SSD_DT_MIN))
    dt_bias = dt0 + jnp.log(-jnp.expm1(-dt0))
    a_log = jnp.log(jax.random.uniform(next(ks), (Ld, 2, SSD_HEADS), f32, 1.0, 16.0))
    FH = HY_FILTER_HIDDEN
    return {
        "x": nrm((BATCH, SEQ, D_MODEL), 1.0),
        "w_in": nrm((Ld, D_MODEL, IN_COLS), D_MODEL ** -0.5),
        "b_in": nrm((Ld, IN_COLS), 0.01),
        "hy_conv_w": nrm((Ld, HY_SHORT_CONV, HY_COLS), HY_SHORT_CONV ** -0.5),
        "hy_conv_b": nrm((Ld, HY_COLS), 0.01),
        "hy_f_w1": nrm((Ld, HY_EMB_DIM, FH), HY_EMB_DIM ** -0.5),
        "hy_f_b1": nrm((Ld, FH), 0.01),
        "hy_f_freq1": ones_noise((Ld, FH)),
        "hy_f_w2": nrm((Ld, FH, FH), FH ** -0.5),
        "hy_f_b2": nrm((Ld, FH), 0.01),
        "hy_f_freq2": ones_noise((Ld, FH)),
        "hy_f_w3": nrm((Ld, FH, FH), FH ** -0.5),
        "hy_f_b3": nrm((Ld, FH), 0.01),
        "hy_f_freq3": ones_noise((Ld, FH)),
        "hy_f_w4": nrm((Ld, FH, HY_ORDER * 2 * D_HY), HY_FILTER_OUT_STD),
        "hy_bias": nrm((Ld, HY_ORDER, D_HY), 0.1),
        "ssd_conv_w": nrm((Ld, SSD_CONV, SSD_CONV_DIM), SSD_CONV ** -0.5),
        "ssd_conv_b": nrm((Ld, SSD_CONV_DIM), 0.01),
        "ssd_dt_bias": dt_bias,
        "ssd_a_log": a_log,
        "ssd_d": ones_noise((Ld, SSD_HEADS)),
        "ssd_norm_w": ones_noise((Ld, D_SSM)),
        "w_hy_branch": xavier(D_HY, D_MODEL, (Ld, D_HY, D_MODEL), DN_BETA),
        "w_ssd_branch": xavier(D_SSM, D_MODEL, (Ld, D_SSM, D_MODEL), DN_BETA),
        "w_out": xavier(D_MODEL, D_MODEL, (Ld, D_MODEL, D_MODEL), DN_BETA),
        "b_out": nrm((Ld, D_MODEL), 0.01),
        "ln1_g": ones_noise((Ld, D_MODEL)),
        "ln1_b": nrm((Ld, D_MODEL), 0.01),
        "router_w": nrm((Ld, D_MODEL, N_EXPERTS), D_MODEL ** -0.5),
        "router_bias": nrm((Ld, N_EXPERTS), 0.01),
        "exp_w_gate": nrm((Ld, N_EXPERTS, D_MODEL, D_EXPERT), D_MODEL ** -0.5),
        "exp_w_up": nrm((Ld, N_EXPERTS, D_MODEL, D_EXPERT), D_MODEL ** -0.5),
        "exp_w_down": xavier(D_EXPERT, D_MODEL, (Ld, N_EXPERTS, D_EXPERT, D_MODEL), DN_BETA),
        "sh_w_gate": nrm((Ld, D_MODEL, D_SHARED), D_MODEL ** -0.5),
        "sh_w_up": nrm((Ld, D_MODEL, D_SHARED), D_MODEL ** -0.5),
        "sh_w_down": xavier(D_SHARED, D_MODEL, (Ld, D_SHARED, D_MODEL), DN_BETA),
        "ln2_g": ones_noise((Ld, D_MODEL)),
        "ln2_b": nrm((Ld, D_MODEL), 0.01),
    }


def reference(x, w_in, b_in, hy_conv_w, hy_conv_b, hy_f_w1, hy_f_b1, hy_f_freq1,
              hy_f_w2, hy_f_b2, hy_f_freq2, hy_f_w3, hy_f_b3, hy_f_freq3, hy_f_w4, hy_bias,
              ssd_conv_w, ssd_conv_b, ssd_dt_bias, ssd_a_log, ssd_d, ssd_norm_w,
              w_hy_branch, w_ssd_branch, w_out, b_out, ln1_g, ln1_b,
              router_w, router_bias, exp_w_gate, exp_w_up, exp_w_down,
              sh_w_gate, sh_w_up, sh_w_down, ln2_g, ln2_b):
    h = x
    for i in range(DEPTH):
        mix = mixer_sublayer(h, w_in[i], b_in[i], hy_conv_w[i], hy_conv_b[i],
                             hy_f_w1[i], hy_f_b1[i], hy_f_freq1[i], hy_f_w2[i], hy_f_b2[i], hy_f_freq2[i],
                             hy_f_w3[i], hy_f_b3[i], hy_f_freq3[i], hy_f_w4[i], hy_bias[i],
                             ssd_conv_w[i], ssd_conv_b[i], ssd_dt_bias[i], ssd_a_log[i], ssd_d[i],
                             ssd_norm_w[i], w_hy_branch[i], w_ssd_branch[i], w_out[i], b_out[i])
        h = layer_norm(DN_ALPHA * h + mix, ln1_g[i], ln1_b[i])
        ffn = moe_sublayer(h, router_w[i], router_bias[i], exp_w_gate[i], exp_w_up[i], exp_w_down[i],
                           sh_w_gate[i], sh_w_up[i], sh_w_down[i])
        h = layer_norm(DN_ALPHA * h + ffn, ln2_g[i], ln2_b[i])
    return h
```

```python
import math
from contextlib import ExitStack
import numpy as np
import ml_dtypes
import concourse.bass as bass
import concourse.mybir as mybir
from concourse.bass_utils import run_bass_kernel_spmd

F32 = mybir.dt.float32
BF16 = mybir.dt.bfloat16
ALU = mybir.AluOpType
AF = mybir.ActivationFunctionType
AX = mybir.AxisListType

ENGS = ("pe", "act", "dve", "pool", "sp")
N_DMA_SEMS = 24
L = 2048
NSEQ = 2
ALPHA = 2.0 ** 0.25
PI = float(np.pi)


class Buf:
    __slots__ = ("name", "w", "r", "x", "mw")

    def __init__(self, name="", x=False):
        self.name = name
        self.w = None
        self.r = []
        self.x = x
        self.mw = []


class Sched:
    def __init__(self, nc, stack):
        self.nc = nc
        self.ops = {e: [] for e in ENGS}
        self.sem = {e: stack.enter_context(nc.semaphore("s_" + e)) for e in ENGS}
        self.dsem = [stack.enter_context(nc.semaphore("d%d" % i)) for i in range(N_DMA_SEMS)]
        self.dcount = [0] * N_DMA_SEMS
        self.dpool = {"sp": list(range(0, 16)), "pool": list(range(16, N_DMA_SEMS))}
        self.dnext = {"sp": 0, "pool": 0}
        self.bar_sem = stack.enter_context(nc.semaphore("bar"))
        self.nbar = 0

    def _deps(self, r, w, eng=None):
        deps = []
        for b in r:
            if b.w is not None:
                deps.append(b.w)
            deps.extend(b.mw)
            if b.x:
                deps.extend(t for t in b.r if not (t[0] == "e" and t[1] == eng))
        for b in w:
            if b.w is not None:
                deps.append(b.w)
            deps.extend(b.r)
        return deps

    def _commit(self, tok, r, w):
        for b in r:
            b.r.append(tok)
        for b in w:
            b.w = tok
            b.r = []

    def op(self, eng, fn, r=(), w=()):
        deps = self._deps(r, w, eng)
        idx = len(self.ops[eng])
        self.ops[eng].append({"fn": fn, "deps": deps, "kind": "c", "needed": False})
        tok = ("e", eng, idx)
        self._commit(tok, r, w)
        return tok

    def dma(self, eng, fn, r=(), w=(), wm=()):
        deps = self._deps(r, w)
        for b in wm:
            if b.w is not None:
                deps.append(b.w)
        pool_ = self.dpool[eng]
        j = pool_[self.dnext[eng] % len(pool_)]
        self.dnext[eng] += 1
        if self.dcount[j] > 0:
            deps.append(("d", j, self.dcount[j] * 16))
        self.dcount[j] += 1
        tok = ("d", j, self.dcount[j] * 16)
        self.ops[eng].append({"fn": fn, "deps": deps, "kind": "d", "dsem": j, "needed": False})
        self._commit(tok, r, w)
        for b in wm:
            b.mw.append(tok)
        return tok

    def barrier(self):
        last = []
        for e in ENGS:
            for i in range(len(self.ops[e]) - 1, -1, -1):
                if self.ops[e][i]["kind"] == "c":
                    last.append(("e", e, i))
                    break
        dl = [("d", j, self.dcount[j] * 16) for j in range(N_DMA_SEMS) if self.dcount[j] > 0]
        self.nbar += 1
        for e in ENGS:
            self.ops[e].append({"fn": None, "deps": last + dl, "kind": "b", "needed": False, "bar": self.nbar})

    def emit(self):
        nc = self.nc
        ops = self.ops
        for e in ENGS:
            for o in ops[e]:
                for d in o["deps"]:
                    if d[0] == "e":
                        ops[d[1]][d[2]]["needed"] = True
        val = {}
        for e in ENGS:
            c = 0
            for i, o in enumerate(ops[e]):
                if o["kind"] == "c" and o["needed"]:
                    c += 1
                    val[(e, i)] = c
        sem, dsem, bar_sem = self.sem, self.dsem, self.bar_sem

        def run(ename, eng):
            waited = {}
            for o in ops[ename]:
                need = {}
                for d in o["deps"]:
                    if d[0] == "e":
                        if d[1] == ename and (ename == "pe" or o["kind"] == "b"):
                            continue
                        key = ("e", d[1])
                        v = val[(d[1], d[2])]
                    else:
                        key = ("d", d[1])
                        v = d[2]
                    if v > need.get(key, 0):
                        need[key] = v
                for key, v in need.items():
                    if waited.get(key, 0) >= v:
                        continue
                    waited[key] = v
                    eng.wait_ge(sem[key[1]] if key[0] == "e" else dsem[key[1]], v)
                if o["kind"] == "b":
                    eng.sem_inc(bar_sem, 1)
                    eng.wait_ge(bar_sem, o["bar"] * len(ENGS))
                    continue
                ins = o["fn"](eng)
                if o["kind"] == "d":
                    ins.then_inc(dsem[o["dsem"]], 16)
                elif o["needed"]:
                    ins.then_inc(sem[ename], 1)

        with nc.Block() as block:
            @block.tensor
            def _(e):
                run("pe", e)

            @block.scalar
            def _(e):
                run("act", e)

            @block.vector
            def _(e):
                run("dve", e)

            @block.gpsimd
            def _(e):
                run("pool", e)

            @block.sync
            def _(e):
                run("sp", e)


class Arena:
    def __init__(self, ap, ncols):
        self.ap = ap
        self.n = ncols
        self.off = 0

    def reset(self):
        self.off = 0

    def alloc(self, nelem, dt=F32, name=""):
        nb = nelem * (2 if dt == BF16 else 4)
        cols = (nb + 3) // 4
        assert self.off + cols <= self.n, ("arena overflow", name, self.off, cols, self.n)
        v = self.ap[:, self.off:self.off + cols]
        self.off += cols
        if dt == BF16:
            v = v.bitcast(BF16)[:, 0:nelem]
        elif dt != F32:
            v = v.bitcast(dt)
        return v, Buf(name)


class Rot:
    def __init__(self, items):
        self.items = items
        self.i = 0

    def next(self):
        it = self.items[self.i % len(self.items)]
        self.i += 1
        return it


CP_HY = 0
CP_XBC = 120
CP_GATE = 344
CP_HYB = 360
NCOL = 376
RP = {}
_o = 0
for _n, _l in [("bz", 2048), ("bdt", 64), ("dtb", 64), ("alog", 64), ("dskip", 2048), ("normw", 2048),
               ("bout", 1024), ("ln1g", 1024), ("ln1b", 1024), ("ln2g", 1024), ("ln2b", 1024), ("rbias", 64)]:
    RP[_n] = (_o, _l)
    _o += _l
NROW = _o
NEXP = 65
SPARSE_MOE = True


def build_program(dbg=None):
    nc = bass.Bass("TRN2", target_bir_lowering=False)

    def din(name, shape, dt=F32):
        return nc.dram_tensor(name, list(shape), dt, kind="ExternalInput").ap()

    def dscr(name, shape, dt=F32):
        kind = "ExternalOutput" if (dbg and name in dbg) else "Internal"
        return nc.dram_tensor(name, list(shape), dt, kind=kind).ap()

    xT = din("xT", [NSEQ, 1024, L])
    x = din("x", [NSEQ, L, 1024])
    w_in = din("w_in", [1024, 11328])
    colpack = din("colpack", [128, NCOL])
    rowpack = din("rowpack", [1, NROW])
    fw1 = din("fw1", [33, 64]); fw2 = din("fw2", [64, 64]); fw3 = din("fw3", [64, 64]); fw4 = din("fw4", [64, 4096])
    fcols = din("fcols", [64, 6])
    w_hyb = din("w_hyb", [1024, 1024]); w_ssdb = din("w_ssdb", [2048, 1024]); w_out = din("w_out", [1024, 1024])
    router_w = din("router_w", [1024, 64])
    ewg = din("ewg", [NEXP, 1024, 256]); ewu = din("ewu", [NEXP, 1024, 256]); ewd = din("ewd", [NEXP, 256, 1024])
    dftc = din("dftc", [128, 16, L], BF16); dfts = din("dfts", [128, 16, L], BF16)
    zT = din("zT", [33, L]); decay = din("decay", [L, 1024])
    altcol = din("altcol", [128, 1], BF16); altrow = din("altrow", [1, L], BF16)
    wsc = din("wsc", [128, 2])
    identf_d = din("identf", [128, 128]); identb_d = din("identb", [128, 128], BF16)
    maskf_d = din("maskf", [128, 128]); maskb_d = din("maskb", [128, 128]); ones_d = din("ones", [128, 128])
    selc_d = din("selc", [32, 4096])
    out = nc.dram_tensor("out", [NSEQ, L, 1024], F32, kind="ExternalOutput").ap()

    KS = dscr("KS", [2, 2, L, 1024]); KN = dscr("KN", [2, 1024])
    HY = dscr("HY", [NSEQ, 3072, L]); SZ = dscr("SZ", [NSEQ, L, 2048]); XBC = dscr("XBC", [NSEQ, 4096, L])
    DTT = dscr("DTT", [NSEQ, L, 64]); G = dscr("G", [NSEQ, 2048, L])
    ZS = dscr("ZS", [NSEQ, 1024, L]); YH = dscr("YH", [NSEQ, 1024, L], BF16); YS = dscr("YS", [NSEQ, 2048, L], BF16)
    CUMF = dscr("CUMF", [NSEQ, 2, 8, 16, 4, 128])
    I32 = mybir.dt.int32
    NTOK = NSEQ * L
    NBLK = 192
    NSLOT = NBLK * 256
    H1B = dscr("H1B", [NTOK + 1, 1024], BF16)
    EWG2 = dscr("EWG2", [64 * 128, 2048], BF16); EWU2 = dscr("EWU2", [64 * 128, 2048], BF16); EWD2 = dscr("EWD2", [64 * 128, 2048], BF16)
    SLOT_TOK = dscr("SLOT_TOK", [NSLOT, 2], I32)
    YSLOT = dscr("YSLOT", [NSLOT, 1024])
    tokid_d = din("tokid", [128, 64], I32); bstart_d = din("bstart", [1, 192]); pcol_d = din("pcol", [128, 1]); strictu_d = din("strictu", [128, 128])
    H1 = dscr("H1", [NSEQ, L, 1024]); H1T = dscr("H1T", [NSEQ, 1024, L], BF16); RW = dscr("RW", [NSEQ, L, NEXP])

    with ExitStack() as st:
        S = Sched(nc, st)
        ACOLS = 53000
        arena_t = st.enter_context(nc.sbuf_tensor("arena", [128, ACOLS], F32))
        AR = Arena(arena_t[:], ACOLS)
        PS = []
        for i in range(8):
            t = st.enter_context(nc.psum_tensor("ps%d" % i, [128, 512], F32))
            PS.append((t[:], Buf("ps%d" % i, x=True)))

        def mm(o, lhsT, rhs, start, stop, r, w):
            S.op("pe", lambda e: e.matmul(o, lhsT, rhs, start=start, stop=stop), r=r, w=w)

        def tr(o, in_, ident, r, w):
            S.op("pe", lambda e: e.transpose(o, in_, ident), r=r, w=w)

        def act(o, in_, func, r, w, bias=0.0, scale=1.0, accum=None):
            if accum is None:
                S.op("act", lambda e: e.activation(out=o, in_=in_, func=func, bias=bias, scale=scale), r=r, w=w)
            else:
                S.op("act", lambda e: e.activation(out=o, in_=in_, func=func, bias=bias, scale=scale, accum_out=accum), r=r, w=w)

        def tt(eng, o, a, b, op, r, w):
            S.op(eng, lambda e: e.tensor_tensor(out=o, in0=a, in1=b, op=op), r=r, w=w)

        def ts(eng, o, a, s1, s2, op0, op1, r, w):
            if op1 is None:
                S.op(eng, lambda e: e.tensor_scalar(out=o, in0=a, scalar1=s1, scalar2=None, op0=op0), r=r, w=w)
            else:
                S.op(eng, lambda e: e.tensor_scalar(out=o, in0=a, scalar1=s1, scalar2=s2, op0=op0, op1=op1), r=r, w=w)

        def stt(eng, o, a, sc, b, op0, op1, r, w, tmp=None):
            if eng == "pool":
                tmp_ap, tmp_b = tmp
                S.op("pool", lambda e: e.tensor_scalar(out=tmp_ap, in0=a, scalar1=sc, scalar2=None, op0=op0), r=list(r) + [tmp_b], w=[tmp_b])
                S.op("pool", lambda e: e.tensor_tensor(out=o, in0=tmp_ap, in1=b, op=op1), r=list(r) + [tmp_b], w=w)
            else:
                S.op(eng, lambda e: e.scalar_tensor_tensor(out=o, in0=a, scalar=sc, in1=b, op0=op0, op1=op1), r=r, w=w)

        def cp(eng, o, a, r, w):
            if eng == "act":
                S.op(eng, lambda e: e.activation(out=o, in_=a, func=AF.Copy), r=r, w=w)
            else:
                S.op(eng, lambda e: e.tensor_copy(out=o, in_=a), r=r, w=w)

        def zero(o, b):
            S.op("dve", lambda e: e.memset(o, 0.0), r=[b], w=[b])

        def ld(o, in_, w, r=(), q="sp"):
            S.dma(q, lambda e: e.dma_start(out=o, in_=in_), r=r, w=w)

        def stor(o, in_, r, q="sp"):
            S.dma(q, lambda e: e.dma_start(out=o, in_=in_), r=r, w=())

        def rowb(name):
            o, l = RP[name]
            return rowpack[:, o:o + l].partition_broadcast(128)

        def wrap_sin(pre, tmp, o, rb, nparts, ncols):
            for _ in range(2):
                ts("dve", tmp, pre, -PI, 2 * PI, ALU.is_lt, ALU.mult, r=rb, w=rb)
                tt("dve", pre, pre, tmp, ALU.add, r=rb, w=rb)
                ts("dve", tmp, pre, PI, -2 * PI, ALU.is_gt, ALU.mult, r=rb, w=rb)
                tt("dve", pre, pre, tmp, ALU.add, r=rb, w=rb)
            ts("dve", pre, pre, -PI, PI, ALU.max, ALU.min, r=rb, w=rb)
            act(o, pre, AF.Sin, r=rb, w=rb)

        def dft_block_loader():
            slots = []
            for i in range(2):
                c, cb_ = AR.alloc(16 * 512, BF16, "dftc%d" % i)
                s_, _ = AR.alloc(16 * 512, BF16, "dfts%d" % i)
                slots.append((c.rearrange("p (a b) -> p a b", a=16), s_.rearrange("p (a b) -> p a b", a=16), cb_))
            rot = Rot(slots)

            def load(idx):
                c, s_, b = rot.next()
                ld(c, dftc[:, :, idx * 512:(idx + 1) * 512], w=[b])
                ld(s_, dfts[:, :, idx * 512:(idx + 1) * 512], w=[b])
                return c, s_, b
            return load

        def stage0():
            AR.reset()
            zt, zb = AR.alloc(L, F32, "zt")
            w1, wb = AR.alloc(64, F32, "w1"); w2, _ = AR.alloc(64, F32); w3, _ = AR.alloc(64, F32)
            w4, _ = AR.alloc(4096, F32); fc, _ = AR.alloc(6, F32); fb, fbb = AR.alloc(3, F32, "fb")
            hA, hAb = AR.alloc(L, F32, "hA"); hB, hBb = AR.alloc(L, F32, "hB")
            pre, preb = AR.alloc(512, F32, "pre"); tmp, _ = AR.alloc(512, F32)
            ac, acb = AR.alloc(1, BF16, "altc")
            wc, wcb = AR.alloc(2, F32, "wsc")
            ld(zt[:33], zT, w=[zb]); ld(w1[:33], fw1, w=[wb]); ld(w2[:64], fw2, w=[wb]); ld(w3[:64], fw3, w=[wb])
            ld(w4[:64], fw4, w=[wb]); ld(fc[:64], fcols, w=[wb]); ld(ac, altcol, w=[acb]); ld(wc, wsc, w=[wcb])
            for i in range(3):
                tt("dve", fb[:64, i:i + 1], fc[:64, 2 * i:2 * i + 1], fc[:64, 2 * i + 1:2 * i + 2], ALU.mult, r=[wb], w=[fbb])
            psr = Rot(PS[0:2])
            chain = [(w1, 33, zt, zb, hA, hAb), (w2, 64, hA, hAb, hB, hBb), (w3, 64, hB, hBb, hA, hAb)]
            for i, (w, K, hin, hinb, hout, houtb) in enumerate(chain):
                for tb in range(4):
                    p, pb = psr.next()
                    mm(p[:64, :], w[:K, 0:64], hin[:K, tb * 512:(tb + 1) * 512], True, True, r=[wb, hinb], w=[pb])
                    act(pre[:64], p[:64, :], AF.Identity, r=[pb, wb, fbb], w=[preb],
                        bias=fb[:64, i:i + 1], scale=fc[:64, 2 * i + 1:2 * i + 2])
                    wrap_sin(pre[:64], tmp[:64], hout[:64, tb * 512:(tb + 1) * 512], [preb, houtb], 64, 512)
            h3, h3b = hA, hAb
            dec, decb = AR.alloc(16 * 512, F32, "dec")
            dec3 = dec.rearrange("p (a b) -> p a b", a=16)
            Aa, Ab = AR.alloc(16 * 512, BF16, "A"); Bm, Bb = AR.alloc(16 * 512, BF16, "Bm")
            A3 = Aa.rearrange("p (a b) -> p a b", a=16); B3 = Bm.rearrange("p (a b) -> p a b", a=16)
            kf, kfb = AR.alloc(512, F32, "kf"); kb_, kbb = AR.alloc(512, F32, "kb")
            kst = [AR.alloc(1024, F32, "kst%d" % i) for i in range(2)]
            kstr = Rot(kst)
            kn, knb = AR.alloc(512, F32, "kn")
            load = dft_block_loader()
            for cb in range(2):
                ld(dec3, decay[:, cb * 512:(cb + 1) * 512].rearrange("(a p) c -> p a c", p=128), w=[decb])
                for o in range(2):
                    colf = o * 2048 + cb * 512
                    colb = o * 2048 + 1024 + cb * 512
                    for t_ in range(16):
                        p0, p0b = PS[2]; p1, p1b = PS[3]
                        mm(p0, h3[:64, t_ * 128:(t_ + 1) * 128], w4[:64, colf:colf + 512], True, True, r=[h3b, wb], w=[p0b])
                        mm(p1, h3[:64, t_ * 128:(t_ + 1) * 128], w4[:64, colb:colb + 512], True, True, r=[h3b, wb], w=[p1b])
                        tt("dve", kf, p0, dec3[:, t_, :], ALU.mult, r=[p0b, decb], w=[kfb])
                        tt("dve", kb_, p1, dec3[:, t_, :], ALU.mult, r=[p1b, decb], w=[kbb])
                        tt("pool", A3[:, t_, :], kf, kb_, ALU.add, r=[kfb, kbb], w=[Ab])
                        tt("pool", B3[:, t_, :], kb_, kf, ALU.subtract, r=[kfb, kbb], w=[Bb])
                    for fbk in range(4):
                        cblk, sblk, dbf = load(fbk)
                        for j in range(4):
                            ft = fbk * 4 + j
                            pR, pRb = PS[4 + (ft % 2)]; pI, pIb = PS[6 + (ft % 2)]
                            for d_ in range(16):
                                mm(pR, cblk[:, d_, j * 128:(j + 1) * 128], A3[:, d_, :], d_ == 0, d_ == 15, r=[dbf, Ab], w=[pRb])
                            for d_ in range(16):
                                mm(pI, sblk[:, d_, j * 128:(j + 1) * 128], B3[:, d_, :], d_ == 0, d_ == 15, r=[dbf, Bb], w=[pIb])
                            k_, k_b = kstr.next()
                            wcol = wc[:, 0:1] if ft == 0 else wc[:, 1:2]
                            act(k_[:, 0:512], pR, AF.Copy, r=[pRb, wcb], w=[k_b], scale=wcol)
                            act(k_[:, 512:1024], pI, AF.Copy, r=[pIb, wcb], w=[k_b], scale=wcol)
                            stor(KS[o, 0, ft * 128:(ft + 1) * 128, cb * 512:(cb + 1) * 512], k_[:, 0:512], r=[k_b])
                            stor(KS[o, 1, ft * 128:(ft + 1) * 128, cb * 512:(cb + 1) * 512], k_[:, 512:1024], r=[k_b])
                    pN, pNb = PS[0]
                    for d_ in range(16):
                        mm(pN[0:1, :], ac[:, 0:1], A3[:, d_, :], d_ == 0, d_ == 15, r=[acb, Ab], w=[pNb])
                    act(kn[0:1, :], pN[0:1, :], AF.Copy, r=[pNb], w=[knb], scale=1.0 / 4096.0)
                    stor(KN[o:o + 1, cb * 512:(cb + 1) * 512], kn[0:1, :], r=[knb])

        def stage1(s):
            AR.reset()
            xtb, xtbb = AR.alloc(8 * L, BF16, "xTb")
            xt3 = xtb.rearrange("p (k t) -> p k t", k=8)
            S.dma("pool", lambda e: e.dma_start(out=xt3, in_=xT[s].rearrange("(k p) t -> p k t", p=128)), w=[xtbb])
            cpk, cpb = AR.alloc(NCOL, F32, "colpack")
            ld(cpk, colpack, w=[cpb])
            wslots = []
            for i in range(3):
                w_, wb_ = AR.alloc(8 * 512, BF16, "wch%d" % i)
                wslots.append((w_.rearrange("p (k c) -> p k c", k=8), wb_))
            wrot = Rot(wslots)
            pbufs = Rot([AR.alloc(L + 8, F32, "P%d" % i) for i in range(2)])
            obufs = Rot([AR.alloc(L, F32, "O%d" % i) for i in range(4)])
            psr = Rot(PS[0:4])
            pst = Rot(PS[4:7])
            ctmp = AR.alloc(L, F32, "convtmp")

            def load_w(col0, n):
                w3, wb_ = wrot.next()
                S.dma("pool", lambda e: e.dma_start(out=w3[:, :, 0:n], in_=w_in.rearrange("(k p) c -> p k c", p=128)[:, :, col0:col0 + n]), w=[wb_])
                return w3, wb_

            def fm_tile(w3, wb_, ct, bias_col, func, P, Pb, poff):
                for tb in range(4):
                    p, pb = psr.next()
                    for k in range(8):
                        mm(p, w3[:, k, ct * 128:(ct + 1) * 128], xt3[:, k, tb * 512:(tb + 1) * 512], k == 0, k == 7, r=[wb_, xtbb], w=[pb])
                    act(P[:, poff + tb * 512: poff + (tb + 1) * 512], p, func, r=[pb, cpb], w=[Pb], bias=bias_col)

            for ch in range(6):
                w3, wb_ = load_w(ch * 512, 512)
                for ct in range(4):
                    j = ch * 4 + ct
                    c0 = CP_HY + 5 * j
                    P, Pb = pbufs.next()
                    S.op("pool", lambda e, P=P: e.memset(P[:, 0:4], 0.0), w=[Pb])
                    S.op("pool", lambda e, P=P: e.memset(P[:, L + 4:L + 8], 0.0), w=[Pb])
                    fm_tile(w3, wb_, ct, cpk[:, c0:c0 + 1], AF.Identity, P, Pb, 4)
                    O, Ob = obufs.next()
                    eng = "dve"
                    ts(eng, O, P[:, 3:3 + L], cpk[:, c0 + 1:c0 + 2], cpk[:, c0 + 4:c0 + 5], ALU.mult, ALU.add, r=[Pb, cpb], w=[Ob])
                    stt(eng, O, P[:, 4:4 + L], cpk[:, c0 + 2:c0 + 3], O, ALU.mult, ALU.add, r=[Pb, cpb, Ob], w=[Ob], tmp=ctmp)
                    stt(eng, O, P[:, 5:5 + L], cpk[:, c0 + 3:c0 + 4], O, ALU.mult, ALU.add, r=[Pb, cpb, Ob], w=[Ob], tmp=ctmp)
                    stor(HY[s, j * 128:(j + 1) * 128, :], O, r=[Ob])
            for ch in range(8):
                w3, wb_ = load_w(5120 + ch * 512, 512)
                for ct in range(4):
                    j = ch * 4 + ct
                    c0 = CP_XBC + 7 * j
                    P, Pb = pbufs.next()
                    S.op("pool", lambda e, P=P: e.memset(P[:, 0:4], 0.0), w=[Pb])
                    S.op("pool", lambda e, P=P: e.memset(P[:, L + 4:L + 8], 0.0), w=[Pb])
                    fm_tile(w3, wb_, ct, cpk[:, c0:c0 + 1], AF.Identity, P, Pb, 4)
                    O, Ob = obufs.next()
                    eng = "dve"
                    ts(eng, O, P[:, 2:2 + L], cpk[:, c0 + 1:c0 + 2], cpk[:, c0 + 6:c0 + 7], ALU.mult, ALU.add, r=[Pb, cpb], w=[Ob])
                    for k in range(1, 5):
                        stt(eng, O, P[:, 2 + k:2 + k + L], cpk[:, c0 + 1 + k:c0 + 2 + k], O, ALU.mult, ALU.add, r=[Pb, cpb, Ob], w=[Ob], tmp=ctmp)
                    act(O, O, AF.Silu, r=[Ob], w=[Ob])
                    stor(XBC[s, j * 128:(j + 1) * 128, :], O, r=[Ob])
            for ch in range(4):
                w3, wb_ = load_w(9280 + ch * 512, 512)
                for ct in range(4):
                    j = ch * 4 + ct
                    O, Ob = obufs.next()
                    fm_tile(w3, wb_, ct, cpk[:, CP_GATE + j:CP_GATE + j + 1], AF.Sigmoid, O, Ob, 0)
                    stor(G[s, j * 128:(j + 1) * 128, :], O, r=[Ob])
            bz, bzb = AR.alloc(2048, F32, "bz")
            ld(bz, rowb("bz"), w=[bzb])
            zo = Rot([AR.alloc(512, F32, "zo%d" % i) for i in range(4)])
            for ch in range(4):
                w3, wb_ = load_w(3072 + ch * 512, 512)
                for t_ in range(16):
                    p, pb = pst.next()
                    for k in range(8):
                        mm(p, xt3[:, k, t_ * 128:(t_ + 1) * 128], w3[:, k, :], k == 0, k == 7, r=[wb_, xtbb], w=[pb])
                    O, Ob = zo.next()
                    tt("dve", O, p, bz[:, ch * 512:(ch + 1) * 512], ALU.add, r=[pb, bzb], w=[Ob])
                    act(O, O, AF.Silu, r=[Ob], w=[Ob])
                    stor(SZ[s, t_ * 128:(t_ + 1) * 128, ch * 512:(ch + 1) * 512], O, r=[Ob])
            bd, bdb = AR.alloc(64, F32, "bd"); bd2, _ = AR.alloc(64, F32)
            ld(bd, rowb("bdt"), w=[bdb]); ld(bd2, rowb("dtb"), w=[bdb])
            tt("dve", bd, bd, bd2, ALU.add, r=[bdb], w=[bdb])
            w3, wb_ = load_w(9216, 64)
            dto, dtob = AR.alloc(16 * 64, F32, "dto")
            dto3 = dto.rearrange("p (a b) -> p a b", a=16)
            for t_ in range(16):
                p, pb = pst.next()
                for k in range(8):
                    mm(p[:, 0:64], xt3[:, k, t_ * 128:(t_ + 1) * 128], w3[:, k, 0:64], k == 0, k == 7, r=[wb_, xtbb], w=[pb])
                tt("dve", dto3[:, t_, :], p[:, 0:64], bd, ALU.add, r=[pb, bdb], w=[dtob])
            act(dto, dto, AF.Exp, r=[dtob], w=[dtob])
            act(dto, dto, AF.Ln, r=[dtob], w=[dtob], bias=1.0)
            stor(DTT[s].rearrange("(a p) c -> p a c", p=128), dto3, r=[dtob])

        def hyena_conv(s, cb, o, src, gate, dst, dst_dt):
            AR.reset()
            cpk, cpb = AR.alloc(NCOL, F32, "colpack")
            ld(cpk, colpack, w=[cpb])
            idb, idbb = AR.alloc(128, BF16, "identb")
            ld(idb, identb_d, w=[idbb])
            ac, acb = AR.alloc(1, BF16, "altc"); arw, arwb = AR.alloc(L, BF16, "altrow")
            ld(ac, altcol, w=[acb]); ld(arw[0:1, :], altrow, w=[arwb])
            knr, knrb = AR.alloc(512, F32, "knr")
            ld(knr[0:1, :], KN[o:o + 1, cb * 512:(cb + 1) * 512], w=[knrb])
            uT, uTb = AR.alloc(16 * 512, BF16, "uT")
            uT3 = uT.rearrange("p (a b) -> p a b", a=16)
            Yr, Yrb = AR.alloc(16 * 512, BF16, "Yr"); Zi, Zib = AR.alloc(16 * 512, BF16, "Zi")
            Yr3 = Yr.rearrange("p (a b) -> p a b", a=16); Zi3 = Zi.rearrange("p (a b) -> p a b", a=16)
            yn, ynb = AR.alloc(512, BF16, "yn")
            load = dft_block_loader()
            kslots = Rot([AR.alloc(2 * 4 * 512, F32, "K%d" % i) for i in range(2)])
            srcs = Rot([AR.alloc(L, F32, "src%d" % i) for i in range(2)])
            sbf, sbfb = AR.alloc(L, BF16, "srcbf")
            t1, t1b = AR.alloc(512, F32, "t1"); t2, t2b = AR.alloc(512, F32, "t2")
            t3, t3b = AR.alloc(512, F32, "t3"); t4, t4b = AR.alloc(512, F32, "t4")
            ep = Rot([(AR.alloc(512, F32, "es%d" % i), AR.alloc(512, F32, "eg%d" % i), AR.alloc(512, dst_dt, "eo%d" % i)) for i in range(3)])
            ptr = Rot(PS[0:2])
            for ct in range(4):
                sr, srb = srcs.next()
                ld(sr, src[ct * 128:(ct + 1) * 128, :], w=[srb])
                cp("dve", sbf, sr, r=[srb], w=[sbfb])
                for g4 in range(4):
                    p, pb = ptr.next()
                    pbv = p.bitcast(BF16)
                    for q in range(4):
                        t_ = g4 * 4 + q
                        tr(pbv[:, q * 128:(q + 1) * 128], sbf[:, t_ * 128:(t_ + 1) * 128], idb, r=[sbfb, idbb], w=[pb])
                    S.op("act", lambda e, g4=g4, ct=ct, pbv=pbv: e.activation(
                        out=uT3[:, g4 * 4:(g4 + 1) * 4, ct * 128:(ct + 1) * 128],
                        in_=pbv[:, 0:512].rearrange("p (a b) -> p a b", a=4), func=AF.Copy), r=[pb], w=[uTb])
            for fbk in range(4):
                cblk, sblk, dbf = load(fbk)
                (kk, kkb) = kslots.next()
                kk4 = kk.rearrange("p (r j c) -> p r j c", r=2, j=4)
                for ri in range(2):
                    ld(kk4[:, ri], KS[o, ri, fbk * 512:(fbk + 1) * 512, cb * 512:(cb + 1) * 512].rearrange("(j p) c -> p j c", p=128), w=[kkb])
                for j in range(4):
                    ft = fbk * 4 + j
                    pC, pCb = PS[2 + (ft % 2)]; pS_, pSb = PS[4 + (ft % 2)]
                    for t_ in range(16):
                        mm(pC, cblk[:, t_, j * 128:(j + 1) * 128], uT3[:, t_, :], t_ == 0, t_ == 15, r=[dbf, uTb], w=[pCb])
                    for t_ in range(16):
                        mm(pS_, sblk[:, t_, j * 128:(j + 1) * 128], uT3[:, t_, :], t_ == 0, t_ == 15, r=[dbf, uTb], w=[pSb])
                    kr = kk4[:, 0, j, :]; ki = kk4[:, 1, j, :]
                    tt("dve", t1, pC, kr, ALU.mult, r=[pCb, kkb], w=[t1b])
                    tt("dve", t2, pS_, ki, ALU.mult, r=[pSb, kkb], w=[t2b])
                    tt("pool", Yr3[:, ft, :], t1, t2, ALU.add, r=[t1b, t2b], w=[Yrb])
                    tt("dve", t3, pS_, kr, ALU.mult, r=[pSb, kkb], w=[t3b])
                    tt("dve", t4, pC, ki, ALU.mult, r=[pCb, kkb], w=[t4b])
                    tt("pool", Zi3[:, ft, :], t3, t4, ALU.subtract, r=[t3b, t4b], w=[Zib])
            pN, pNb = PS[6]
            for t_ in range(16):
                mm(pN[0:1, :], ac[:, 0:1], uT3[:, t_, :], t_ == 0, t_ == 15, r=[acb, uTb], w=[pNb])
            tt("dve", yn[0:1, :], pN[0:1, :], knr[0:1, :], ALU.mult, r=[pNb, knrb], w=[ynb])
            pinv = Rot([PS[6], PS[7], PS[0], PS[1]])
            for tb in range(4):
                cblk, sblk, dbf = load(tb)
                for ct in range(4):
                    p, pb = pinv.next()
                    for ft in range(16):
                        mm(p, Yr3[:, ft, ct * 128:(ct + 1) * 128], cblk[:, ft, :], ft == 0, False, r=[Yrb, dbf], w=[pb])
                    for ft in range(16):
                        mm(p, Zi3[:, ft, ct * 128:(ct + 1) * 128], sblk[:, ft, :], False, False, r=[Zib, dbf], w=[pb])
                    mm(p, yn[0:1, ct * 128:(ct + 1) * 128], arw[0:1, tb * 512:(tb + 1) * 512], False, True, r=[ynb, arwb], w=[pb])
                    (es, esb), (eg, egb), (eo, eob) = ep.next()
                    ld(es, src[ct * 128:(ct + 1) * 128, tb * 512:(tb + 1) * 512], w=[esb])
                    ld(eg, gate[ct * 128:(ct + 1) * 128, tb * 512:(tb + 1) * 512], w=[egb])
                    bcol = cpk[:, CP_HYB + o * 8 + cb * 4 + ct: CP_HYB + o * 8 + cb * 4 + ct + 1]
                    stt("dve", es, es, bcol, p, ALU.mult, ALU.add, r=[esb, cpb, pb], w=[esb])
                    tt("pool", eo, es, eg, ALU.mult, r=[esb, egb], w=[eob])
                    stor(dst[ct * 128:(ct + 1) * 128, tb * 512:(tb + 1) * 512], eo, r=[eob])

        def stage3(s):
            AR.reset()
            idf, idfb = AR.alloc(128, F32, "identf"); idb, idbb = AR.alloc(128, BF16, "identb")
            mkf, mkb_ = AR.alloc(128, F32, "maskf"); mkb, _ = AR.alloc(128, F32, "maskb"); on, _ = AR.alloc(128, F32, "ones")
            sel, selb = AR.alloc(512, F32, "sel")
            ld(idf, identf_d, w=[idfb]); ld(idb, identb_d, w=[idbb]); ld(mkf, maskf_d, w=[mkb_]); ld(mkb, maskb_d, w=[mkb_])
            ld(on, ones_d, w=[mkb_])
            sel3 = sel.rearrange("p (h m) -> p h m", h=4)
            dtt, dttb = AR.alloc(16 * 64, F32, "dtt"); dtt3 = dtt.rearrange("p (a b) -> p a b", a=16)
            ld(dtt3, DTT[s].rearrange("(a p) c -> p a c", p=128), w=[dttb])
            nA, nAb = AR.alloc(64, F32, "negA")
            ld(nA, rowb("alog"), w=[nAb])
            act(nA, nA, AF.Exp, r=[nAb], w=[nAb])
            ts("dve", nA, nA, -1.0, None, ALU.mult, None, r=[nAb], w=[nAb])
            at_, atb = AR.alloc(16 * 64, F32, "a_tok"); at3 = at_.rearrange("p (a b) -> p a b", a=16)
            tt("dve", at3, dtt3, nA[:, None, :].to_broadcast([128, 16, 64]), ALU.mult, r=[dttb, nAb], w=[atb])
            cum, cumb = AR.alloc(16 * 64, F32, "cum"); cum3 = cum.rearrange("p (a b) -> p a b", a=16)
            tot, totb = AR.alloc(16 * 64, F32, "tot"); tot3 = tot.rearrange("p (a b) -> p a b", a=16)
            cFf, cFfb = AR.alloc(L, F32, "cumFf"); cFb, cFbb = AR.alloc(L, F32, "cumFb")
            psr = Rot(PS[0:4])
            for c in range(16):
                p, pb = psr.next()
                mm(p[:, 0:32], mkf, at3[:, c, 0:32], True, True, r=[mkb_, atb], w=[pb])
                mm(p[:, 32:64], mkb, at3[:, c, 32:64], True, True, r=[mkb_, atb], w=[pb])
                mm(p[:, 64:128], on, at3[:, c, :], True, True, r=[mkb_, atb], w=[pb])
                cp("dve", cum3[:, c, :], p[:, 0:64], r=[pb], w=[cumb])
                cp("dve", tot3[:, c, :], p[:, 64:128], r=[pb], w=[totb])
                p2, p2b = psr.next()
                mm(p2[0:32, 0:128], at3[:, c, 0:32], mkf, True, True, r=[mkb_, atb], w=[p2b])
                mm(p2[0:32, 128:256], at3[:, c, 32:64], mkb, True, True, r=[mkb_, atb], w=[p2b])
                cp("dve", cFf[0:32, c * 128:(c + 1) * 128], p2[0:32, 0:128], r=[p2b], w=[cFfb])
                cp("dve", cFb[0:32, c * 128:(c + 1) * 128], p2[0:32, 128:256], r=[p2b], w=[cFbb])
            cumDB = [[Buf("cumf_dram%d_%d" % (d, g_)) for g_ in range(8)] for d in range(2)]
            for d, (cF_, cF_b) in enumerate([(cFf, cFfb), (cFb, cFbb)]):
                for g_ in range(8):
                    S.dma("sp", lambda e, d=d, g_=g_, cF_=cF_: e.dma_start(
                        out=CUMF[s, d, g_].rearrange("c h l -> h c l"),
                        in_=cF_[g_ * 4:(g_ + 1) * 4, :].rearrange("p (c l) -> p c l", c=16)), r=[cF_b], w=[cumDB[d][g_]])
            ncum, ncumb = AR.alloc(16 * 64, F32, "ncum")
            ts("dve", ncum, cum, -1.0, None, ALU.mult, None, r=[cumb], w=[ncumb])
            ncum3 = ncum.rearrange("p (a b) -> p a b", a=16)
            dte, dteb = AR.alloc(16 * 64, F32, "dte"); dte3 = dte.rearrange("p (a b) -> p a b", a=16)
            tt("dve", dte, tot, cum, ALU.subtract, r=[totb, cumb], w=[dteb])
            act(dte, dte, AF.Exp, r=[dteb], w=[dteb])
            tt("dve", dte, dte, dtt, ALU.mult, r=[dteb, dttb], w=[dteb])
            cdec, cdecb = AR.alloc(16 * 64, F32, "cdec"); cdec3 = cdec.rearrange("p (a b) -> p a b", a=16)
            act(cdec, tot, AF.Exp, r=[totb], w=[cdecb])
            dsk, dskb = AR.alloc(256, F32, "dskip"); nw, nwb = AR.alloc(256, F32, "normw")
            xsF = [AR.alloc(L, F32, "xsF%d" % i) for i in range(2)]
            BTs = [AR.alloc(L, BF16, "BT%d" % i) for i in range(2)]; CTs2 = [AR.alloc(L, BF16, "CT%d" % i) for i in range(2)]
            xtok, xtokb = AR.alloc(16 * 256, F32, "xtok"); xtok3 = xtok.rearrange("p (a b) -> p a b", a=16)
            btok, btokb = AR.alloc(16 * 128, BF16, "btok"); btok3 = btok.rearrange("p (a b) -> p a b", a=16)
            stf = [AR.alloc(16 * 256, F32, "st%d" % d) for d in range(2)]
            prv = [AR.alloc(16 * 256, BF16, "prv%d" % d) for d in range(2)]
            rtmps = [AR.alloc(256, F32, "rtmp%d" % d) for d in range(2)]
            rowbuf = Rot([AR.alloc(512, F32, "rowb%d" % i) for i in range(6)])
            xe = Rot([AR.alloc(256, BF16, "xe%d" % i) for i in range(4)])
            xd = Rot([AR.alloc(256, BF16, "xd%d" % i) for i in range(4)])
            cbm = [[AR.alloc(128, F32, "cbm%d_%d" % (par, d)) for d in range(2)] for par in range(2)]
            dif = Rot([AR.alloc(512, F32, "dif%d" % i) for i in range(2)])
            Dm = Rot([AR.alloc(512, F32, "D%d" % i) for i in range(2)])
            MT = Rot([AR.alloc(512, BF16, "MT%d" % i) for i in range(4)])
            Eb = Rot([AR.alloc(512, F32, "Eb%d" % i) for i in range(2)])
            CTs = Rot([AR.alloc(512, BF16, "CTs%d" % i) for i in range(4)])
            yv = Rot([AR.alloc(256, F32, "y%d" % i) for i in range(2)])
            szr = Rot([AR.alloc(256, F32, "sz%d" % i) for i in range(4)])
            ysq, ysqb = AR.alloc(256, F32, "ysq")
            st4r = Rot([AR.alloc(4, F32, "stat%d" % i) for i in range(2)])
            ybf = Rot([AR.alloc(256, BF16, "ybf%d" % i) for i in range(2)])
            yso = Rot([AR.alloc(256, BF16, "yso%d" % i) for i in range(4)])
            ptr = Rot(PS[0:4] + PS[6:8])
            misc = [PS[4][0], PS[5][0]]
            miscB = [(PS[4][1],) * 3, (PS[5][1],) * 3]
            h4 = lambda ap: ap.rearrange("p (h q) -> p h q", h=4)
            def load_xs(g_):
                for i in range(2):
                    ld(xsF[i][0], XBC[s, g_ * 256 + i * 128: g_ * 256 + (i + 1) * 128, :], w=[xsF[i][1]])

            def load_bc(g_):
                bt_, btb_ = BTs[g_ % 2]; ct_, ctb_ = CTs2[g_ % 2]
                S.dma("pool", lambda e, g_=g_, bt_=bt_: e.dma_start(out=bt_, in_=XBC[s, 2048 + g_ * 128: 2048 + (g_ + 1) * 128, :]), w=[btb_])
                S.dma("pool", lambda e, g_=g_, ct_=ct_: e.dma_start(out=ct_, in_=XBC[s, 3072 + g_ * 128: 3072 + (g_ + 1) * 128, :]), w=[ctb_])

            load_xs(0)
            load_bc(0)
            for g in range(8):
                BT, BTb = BTs[g % 2]; CT, CTb = CTs2[g % 2]
                o_, l_ = RP["dskip"]
                ld(dsk, rowpack[:, o_ + g * 256:o_ + (g + 1) * 256].partition_broadcast(128), w=[dskb])
                o_, l_ = RP["normw"]
                ld(nw, rowpack[:, o_ + g * 256:o_ + (g + 1) * 256].partition_broadcast(128), w=[nwb])
                for c in range(16):
                    p, pb = ptr.next()
                    for i in range(2):
                        tr(p[:, i * 128:(i + 1) * 128], xsF[i][0][:, c * 128:(c + 1) * 128], idf, r=[xsF[i][1], idfb], w=[pb])
                    cp("act", xtok3[:, c, :], p[:, 0:256], r=[pb], w=[xtokb])
                for c4 in range(4):
                    p, pb = ptr.next()
                    pbv = p.bitcast(BF16)
                    for q in range(4):
                        c = c4 * 4 + q
                        tr(pbv[:, q * 128:(q + 1) * 128], BT[:, c * 128:(c + 1) * 128], idb, r=[BTb, idbb], w=[pb])
                    S.op("act", lambda e, c4=c4, pbv=pbv: e.activation(out=btok3[:, c4 * 4:(c4 + 1) * 4, :],
                         in_=pbv[:, 0:512].rearrange("p (a b) -> p a b", a=4), func=AF.Copy), r=[pb], w=[btokb])
                if g + 1 < 8:
                    load_xs(g + 1)
                for c in range(16):
                    for d in range(2):
                        (xe_, xeb) = xe.next()
                        tt("pool" if d == 0 else "dve", h4(xe_), h4(xtok3[:, c, :]),
                           dte3[:, c, d * 32 + g * 4: d * 32 + g * 4 + 4].unsqueeze(2).to_broadcast([128, 4, 64]), ALU.mult,
                           r=[xtokb, dteb], w=[xeb])
                        p, pb = ptr.next()
                        mm(p[:, 0:256], btok3[:, c, :], xe_, True, True, r=[btokb, xeb], w=[pb])
                        cp("act", stf[d][0][:, c * 256:(c + 1) * 256], p[:, 0:256], r=[pb], w=[stf[d][1]])
                for k in range(1, 15):
                    for d in range(2):
                        c = k if d == 0 else 15 - k
                        cprev = c - 1 if d == 0 else c + 1
                        arr, arrb = stf[d]
                        rt, rtb = rtmps[d]
                        tt("dve", h4(rt), h4(arr[:, cprev * 256:(cprev + 1) * 256]),
                           cdec3[:, c, d * 32 + g * 4: d * 32 + g * 4 + 4].unsqueeze(2).to_broadcast([128, 4, 64]), ALU.mult,
                           r=[arrb, cdecb, rtb], w=[rtb])
                        tt("dve", arr[:, c * 256:(c + 1) * 256], rt, arr[:, c * 256:(c + 1) * 256], ALU.add, r=[rtb, arrb], w=[arrb])
                S.op("pool", lambda e: e.memset(prv[0][0][:, 0:256], 0.0), r=[prv[0][1]], w=[prv[0][1]])
                S.op("pool", lambda e: e.memset(prv[1][0][:, 15 * 256:16 * 256], 0.0), r=[prv[1][1]], w=[prv[1][1]])
                cp("act", prv[0][0][:, 256:4096], stf[0][0][:, 0:3840], r=[stf[0][1]], w=[prv[0][1]])
                cp("act", prv[1][0][:, 0:3840], stf[1][0][:, 256:4096], r=[stf[1][1]], w=[prv[1][1]])

                if g + 1 < 8:
                    load_bc(g + 1)
                it = {}

                def front_a(c):
                    par = c % 2
                    pm = misc[par]; mB = miscB[par][0]
                    d_ = {}
                    mm(pm[:, 0:128], BT[:, c * 128:(c + 1) * 128], CT[:, c * 128:(c + 1) * 128], True, True, r=[BTb, CTb], w=[mB])
                    tt("dve", cbm[par][0][0], pm[:, 0:128], mkf, ALU.mult, r=[mB, mkb_], w=[cbm[par][0][1]])
                    tt("dve", cbm[par][1][0], pm[:, 0:128], mkb, ALU.mult, r=[mB, mkb_], w=[cbm[par][1][1]])
                    d_["dirs"] = []
                    for d in range(2):
                        pr, prb = rowbuf.next()
                        ld(pr, CUMF[s, d, g, c:c + 1].rearrange("o h l -> o (h l)").partition_broadcast(128), w=[prb], r=[cumDB[d][g]])
                        df, dfb = dif.next()
                        c0 = d * 32 + g * 4
                        tt("dve", df.rearrange("p (h l) -> p h l", h=4), pr.rearrange("p (h l) -> p h l", h=4),
                           ncum3[:, c, c0:c0 + 4].unsqueeze(2).to_broadcast([128, 4, 128]), ALU.add, r=[prb, ncumb, dfb], w=[dfb])
                        D_, Db = Dm.next()
                        act(D_, df, AF.Exp, r=[dfb], w=[Db])
                        E_, Ebb = Eb.next()
                        act(E_, pr, AF.Exp, r=[prb], w=[Ebb])
                        xd_, xdb = xd.next()
                        tt("pool", h4(xd_), h4(xtok3[:, c, :]),
                           dtt3[:, c, c0:c0 + 4].unsqueeze(2).to_broadcast([128, 4, 64]), ALU.mult, r=[xtokb, dttb], w=[xdb])
                        d_["dirs"].append((D_, Db, E_, Ebb, xd_, xdb, d))
                    y_, yb_ = yv.next()
                    sz, szb = szr.next()
                    ld(sz, SZ[s, c * 128:(c + 1) * 128, g * 256:(g + 1) * 256], w=[szb])
                    tt("pool", y_, xtok3[:, c, :], dsk, ALU.mult, r=[xtokb, dskb], w=[yb_])
                    d_["y"] = (y_, yb_, sz, szb)
                    it[c] = d_

                def front_b(c):
                    par = c % 2
                    d_ = it[c]
                    mts = []
                    for (D_, Db, E_, Ebb, xd_, xdb, d) in d_["dirs"]:
                        mt, mtb = MT.next()
                        stt("dve", mt.rearrange("p (h l) -> p h l", h=4), D_.rearrange("p (h l) -> p h l", h=4), 1.0,
                            cbm[par][d][0][:, None, :].to_broadcast([128, 4, 128]), ALU.min, ALU.mult, r=[Db, cbm[par][d][1]], w=[mtb])
                        cs, csb = CTs.next()
                        tt("pool", cs.rearrange("p (h l) -> p h l", h=4), E_.rearrange("p (h l) -> p h l", h=4),
                           CT[:, None, c * 128:(c + 1) * 128].to_broadcast([128, 4, 128]), ALU.mult, r=[Ebb, CTb], w=[csb])
                        mts.append((mt, mtb, cs, csb, xd_, xdb, d))
                    d_["mts"] = mts

                def back_pe(c):
                    par = c % 2
                    pm = misc[par]; mB = miscB[par][0]
                    yp = pm[:, 128:384]
                    for h in range(4):
                        for k, (mt, mtb, cs, csb, xd_, xdb, d) in enumerate(it[c]["mts"]):
                            mm(yp[:, h * 64:(h + 1) * 64], mt[:, h * 128:(h + 1) * 128], xd_[:, h * 64:(h + 1) * 64], k == 0, False,
                               r=[mtb, xdb], w=[mB])
                            mm(yp[:, h * 64:(h + 1) * 64], cs[:, h * 128:(h + 1) * 128], prv[d][0][:, c * 256 + h * 64: c * 256 + (h + 1) * 64],
                               False, k == 1, r=[csb, prv[d][1]], w=[mB])

                def back_a(c):
                    par = c % 2
                    pm = misc[par]; mB = miscB[par][0]
                    yp = pm[:, 128:384]
                    y_, yb_, sz, szb = it[c]["y"]
                    st4, st4b = st4r.next()
                    tt("dve", y_, y_, yp, ALU.add, r=[yb_, mB], w=[yb_])
                    tt("dve", y_, y_, sz, ALU.mult, r=[yb_, szb], w=[yb_])
                    tt("dve", ysq, y_, y_, ALU.mult, r=[yb_, ysqb], w=[ysqb])
                    S.op("dve", lambda e, st4=st4: e.reduce_sum(out=st4[:, 0:1], in_=ysq, axis=AX.X), r=[ysqb, st4b], w=[st4b])
                    act(st4[:, 1:2], st4[:, 0:1], AF.Ln, r=[st4b], w=[st4b], bias=1e-5, scale=1.0 / 256.0)
                    act(st4[:, 2:3], st4[:, 1:2], AF.Exp, r=[st4b], w=[st4b], scale=-0.5)
                    it[c]["st4"] = (st4, st4b)

                def back_b(c):
                    y_, yb_, sz, szb = it[c]["y"]
                    st4, st4b = it[c]["st4"]
                    yb16, yb16b = ybf.next()
                    stt("dve", yb16, y_, st4[:, 2:3], nw, ALU.mult, ALU.mult, r=[yb_, st4b, nwb], w=[yb16b])
                    it[c]["yb16"] = (yb16, yb16b)

                def back_t(c):
                    yb16, yb16b = it[c]["yb16"]
                    pt, ptb = ptr.next()
                    pbv = pt.bitcast(BF16)
                    for i in range(2):
                        tr(pbv[:, i * 128:(i + 1) * 128], yb16[:, i * 128:(i + 1) * 128], idb, r=[yb16b, idbb], w=[ptb])
                    yo, yob = yso.next()
                    cp("act", yo, pbv[:, 0:256], r=[ptb], w=[yob])
                    stor(YS[s, g * 256:(g + 1) * 256, c * 128:(c + 1) * 128].rearrange("(i p) t -> p i t", p=128),
                         yo.rearrange("p (i t) -> p i t", i=2), r=[yob])
                    del it[c]

                front_a(0)
                front_b(0)
                for c in range(16):
                    if c + 1 < 16:
                        front_a(c + 1)
                    back_pe(c)
                    if c >= 1:
                        back_t(c - 1)
                    back_a(c)
                    if c + 1 < 16:
                        front_b(c + 1)
                    back_b(c)
                back_t(15)

        def layer_norm_tile(r_, rb, g_row, b_row, gb, o_, ob, st4, st4b, sq, sqb):
            S.op("dve", lambda e: e.reduce_sum(out=st4[:, 0:1], in_=r_, axis=AX.X), r=[rb], w=[st4b])
            ts("dve", st4[:, 1:2], st4[:, 0:1], -1.0 / 1024.0, None, ALU.mult, None, r=[st4b], w=[st4b])
            ts("dve", r_, r_, st4[:, 1:2], None, ALU.add, None, r=[rb, st4b], w=[rb])
            tt("dve", sq, r_, r_, ALU.mult, r=[rb, sqb], w=[sqb])
            S.op("dve", lambda e: e.reduce_sum(out=st4[:, 2:3], in_=sq, axis=AX.X), r=[sqb, st4b], w=[st4b])
            act(st4[:, 3:4], st4[:, 2:3], AF.Ln, r=[st4b], w=[st4b], bias=1e-5, scale=1.0 / 1024.0)
            act(st4[:, 4:5], st4[:, 3:4], AF.Exp, r=[st4b], w=[st4b], scale=-0.5)
            stt("dve", o_, r_, st4[:, 4:5], g_row, ALU.mult, ALU.mult, r=[rb, st4b, gb], w=[ob])
            tt("dve", o_, o_, b_row, ALU.add, r=[ob, gb], w=[ob])

        def stage4(s):
            AR.reset()
            whb, whbb = AR.alloc(8 * 1024, BF16, "whb"); wsb, _ = AR.alloc(16 * 1024, BF16, "wsb"); wo, _ = AR.alloc(8 * 1024, BF16, "wo")
            whb3 = whb.rearrange("p (k c) -> p k c", k=8); wsb3 = wsb.rearrange("p (k c) -> p k c", k=16); wo3 = wo.rearrange("p (k c) -> p k c", k=8)
            S.dma("pool", lambda e: e.dma_start(out=whb3, in_=w_hyb.rearrange("(k p) c -> p k c", p=128)), w=[whbb])
            S.dma("pool", lambda e: e.dma_start(out=wsb3, in_=w_ssdb.rearrange("(k p) c -> p k c", p=128)), w=[whbb])
            S.dma("pool", lambda e: e.dma_start(out=wo3, in_=w_out.rearrange("(k p) c -> p k c", p=128)), w=[whbb])
            rwt, rwtb = AR.alloc(8 * 64, F32, "routerw"); rw3 = rwt.rearrange("p (k c) -> p k c", k=8)
            ld(rw3, router_w.rearrange("(k p) c -> p k c", p=128), w=[rwtb])
            idf, idfb = AR.alloc(128, F32, "identf")
            ld(idf, identf_d, w=[idfb])
            rows, rowsb = AR.alloc(3 * 1024 + 64, F32, "rows")
            ld(rows[:, 0:1024], rowb("bout"), w=[rowsb]); ld(rows[:, 1024:2048], rowb("ln1g"), w=[rowsb])
            ld(rows[:, 2048:3072], rowb("ln1b"), w=[rowsb]); ld(rows[:, 3072:3136], rowb("rbias"), w=[rowsb])
            yh, yhb = AR.alloc(8 * 512, BF16, "yh"); ys, ysb = AR.alloc(16 * 512, BF16, "ys")
            yh3 = yh.rearrange("p (k t) -> p k t", k=8); ys3 = ys.rearrange("p (k t) -> p k t", k=16)
            gt = Rot([(AR.alloc(512, F32, "gh%d" % i), AR.alloc(512, F32, "gs%d" % i)) for i in range(4)])
            ta, tab = AR.alloc(512, F32, "ta"); tb_, tbb = AR.alloc(512, F32, "tb")
            mT, mTb = AR.alloc(8 * 512, BF16, "mT"); mT3 = mT.rearrange("p (k t) -> p k t", k=8)
            xr = Rot([AR.alloc(1024, F32, "xr%d" % i) for i in range(2)])
            ho = Rot([AR.alloc(1024, F32, "ho%d" % i) for i in range(2)])
            sq, sqb = AR.alloc(1024, F32, "sq"); st8, st8b = AR.alloc(8, F32, "st8")
            hT32, hT32b = AR.alloc(8 * 128, F32, "hT32"); hT16 = Rot([AR.alloc(8 * 128, BF16, "hT16_%d" % i) for i in range(2)])
            sc, scb = AR.alloc(64, F32, "sc"); bi, bib = AR.alloc(64, F32, "bi"); m8, m8b = AR.alloc(8, F32, "m8")
            gs, gsb = AR.alloc(8, F32, "gs"); gm, gmb = AR.alloc(8, F32, "gm"); pen, penb = AR.alloc(8, F32, "pen")
            msk, mskb = AR.alloc(64, F32, "msk"); rwo = Rot([AR.alloc(NEXP, F32, "rwo%d" % i) for i in range(2)])
            hrow16 = Rot([AR.alloc(1024, BF16, "hrow16_%d" % i) for i in range(2)])
            wcast = Rot([AR.alloc(2048, BF16, "wcast%d" % i) for i in range(8)])
            if s == 0:
                zr, zrb = AR.alloc(1024, BF16, "zerorow")
                S.op("pool", lambda e: e.memset(zr[0:1, :], 0.0), w=[zrb])
                stor(H1B[NTOK:NTOK + 1, :], zr[0:1, :], r=[zrb])
            pbr = Rot(PS[0:4]); pot = Rot(PS[4:6]); ptr = Rot(PS[6:8])
            for tb in range(4):
                ld(yh3, YH[s, :, tb * 512:(tb + 1) * 512].rearrange("(k p) t -> p k t", p=128), w=[yhb])
                ld(ys3, YS[s, :, tb * 512:(tb + 1) * 512].rearrange("(k p) t -> p k t", p=128), w=[ysb])
                for dt_ in range(8):
                    (gh, ghb), (gs_, gsb_) = gt.next()
                    ld(gh, G[s, dt_ * 128:(dt_ + 1) * 128, tb * 512:(tb + 1) * 512], w=[ghb])
                    ld(gs_, G[s, 1024 + dt_ * 128:1024 + (dt_ + 1) * 128, tb * 512:(tb + 1) * 512], w=[gsb_])
                    pH, pHb = pbr.next()
                    for k in range(8):
                        mm(pH, whb3[:, k, dt_ * 128:(dt_ + 1) * 128], yh3[:, k, :], k == 0, k == 7, r=[whbb, yhb], w=[pHb])
                    pS_, pSb = pbr.next()
                    for k in range(16):
                        mm(pS_, wsb3[:, k, dt_ * 128:(dt_ + 1) * 128], ys3[:, k, :], k == 0, k == 15, r=[whbb, ysb], w=[pSb])
                    tt("dve", ta, pH, gh, ALU.mult, r=[pHb, ghb, tab], w=[tab])
                    tt("dve", tb_, pS_, gs_, ALU.mult, r=[pSb, gsb_, tbb], w=[tbb])
                    tt("dve", mT3[:, dt_, :], ta, tb_, ALU.add, r=[tab, tbb], w=[mTb])
                for q in range(4):
                    t_ = tb * 4 + q
                    xr_, xrb = xr.next()
                    ld(xr_, x[s, t_ * 128:(t_ + 1) * 128, :], w=[xrb])
                    for dh in range(2):
                        p, pb = pot.next()
                        for k in range(8):
                            mm(p, mT3[:, k, q * 128:(q + 1) * 128], wo3[:, k, dh * 512:(dh + 1) * 512], k == 0, k == 7, r=[mTb, whbb], w=[pb])
                        stt("dve", xr_[:, dh * 512:(dh + 1) * 512], xr_[:, dh * 512:(dh + 1) * 512], ALPHA, p, ALU.mult, ALU.add, r=[xrb, pb], w=[xrb])
                    tt("dve", xr_, xr_, rows[:, 0:1024], ALU.add, r=[xrb, rowsb], w=[xrb])
                    h_, hb_ = ho.next()
                    layer_norm_tile(xr_, xrb, rows[:, 1024:2048], rows[:, 2048:3072], rowsb, h_, hb_, st8, st8b, sq, sqb)
                    stor(H1[s, t_ * 128:(t_ + 1) * 128, :], h_, r=[hb_])
                    hb16, hb16b = hrow16.next()
                    cp("act", hb16, h_, r=[hb_], w=[hb16b])
                    stor(H1B[s * L + t_ * 128: s * L + (t_ + 1) * 128, :], hb16, r=[hb16b])
                    for e_ in (s * 32 + t_ * 2, s * 32 + t_ * 2 + 1):
                        for (src_, dst_, kk, pat) in ((ewg, EWG2, 8, "(k p) f -> p k f"), (ewu, EWU2, 8, "(k p) f -> p k f"), (ewd, EWD2, 2, "(k p) d -> p k d")):
                            wt, wtb = wcast.next()
                            S.dma("pool", lambda e, wt=wt, src_=src_, e_=e_, kk=kk, pat=pat: e.dma_start(
                                out=wt.rearrange("p (k c) -> p k c", k=kk), in_=src_[e_].rearrange(pat, p=128)), w=[wtb])
                            stor(dst_[e_ * 128:(e_ + 1) * 128, :], wt, r=[wtb])
                    h16, h16b = hT16.next()
                    for k4 in range(2):
                        p, pb = ptr.next()
                        for k in range(4):
                            kk = k4 * 4 + k
                            tr(p[:, k * 128:(k + 1) * 128], h_[:, kk * 128:(kk + 1) * 128], idf, r=[hb_, idfb], w=[pb])
                        cp("act", hT32[:, k4 * 512:(k4 + 1) * 512], p, r=[pb], w=[hT32b])
                        cp("act", h16[:, k4 * 512:(k4 + 1) * 512], p, r=[pb], w=[h16b])
                    stor(H1T[s, :, t_ * 128:(t_ + 1) * 128].rearrange("(k p) t -> p k t", p=128), h16.rearrange("p (k t) -> p k t", k=8), r=[h16b])
                    p, pb = ptr.next()
                    for k in range(8):
                        mm(p[:, 0:64], hT32[:, k * 128:(k + 1) * 128], rw3[:, k, :], k == 0, k == 7, r=[hT32b, rwtb], w=[pb])
                    act(sc, p[:, 0:64], AF.Exp, r=[pb], w=[scb], scale=-1.0)
                    ts("dve", sc, sc, 1.0, None, ALU.add, None, r=[scb], w=[scb])
                    S.op("dve", lambda e: e.reciprocal(out=sc, in_=sc), r=[scb], w=[scb])
                    tt("dve", bi, sc, rows[:, 3072:3136], ALU.add, r=[scb, rowsb], w=[bib])
                    for g in range(8):
                        S.op("dve", lambda e, g=g: e.max(out=m8, in_=bi[:, g * 8:(g + 1) * 8]), r=[bib, m8b], w=[m8b])
                        tt("dve", gs[:, g:g + 1], m8[:, 0:1], m8[:, 1:2], ALU.add, r=[m8b], w=[gsb])
                    S.op("dve", lambda e: e.max(out=m8, in_=gs), r=[gsb, m8b], w=[m8b])
                    ts("dve", gm, gs, m8[:, 3:4], None, ALU.is_ge, None, r=[gsb, m8b], w=[gmb])
                    ts("dve", pen, gm, -1.0, 1e9, ALU.add, ALU.mult, r=[gmb], w=[penb])
                    tt("dve", msk.rearrange("p (g e) -> p g e", g=8), bi.rearrange("p (g e) -> p g e", g=8),
                       gm.unsqueeze(2).to_broadcast([128, 8, 8]), ALU.mult, r=[bib, gmb], w=[mskb])
                    tt("dve", msk.rearrange("p (g e) -> p g e", g=8), msk.rearrange("p (g e) -> p g e", g=8),
                       pen.unsqueeze(2).to_broadcast([128, 8, 8]), ALU.add, r=[mskb, penb], w=[mskb])
                    S.op("dve", lambda e: e.max(out=m8, in_=msk), r=[mskb, m8b], w=[m8b])
                    ts("dve", msk, msk, m8[:, 7:8], None, ALU.is_ge, None, r=[mskb, m8b], w=[mskb])
                    rw_, rwb_ = rwo.next()
                    tt("dve", rw_[:, 0:64], sc, msk, ALU.mult, r=[scb, mskb], w=[rwb_])
                    S.op("dve", lambda e, rw_=rw_: e.reduce_sum(out=st8[:, 5:6], in_=rw_[:, 0:64], axis=AX.X), r=[rwb_], w=[st8b])
                    S.op("dve", lambda e: e.reciprocal(out=st8[:, 6:7], in_=st8[:, 5:6]), r=[st8b], w=[st8b])
                    ts("dve", rw_[:, 0:64], rw_[:, 0:64], st8[:, 6:7], 2.5, ALU.mult, ALU.mult, r=[rwb_, st8b], w=[rwb_])
                    S.op("pool", lambda e, rw_=rw_: e.memset(rw_[:, 64:65], 1.0), r=[rwb_], w=[rwb_])
                    stor(RW[s, t_ * 128:(t_ + 1) * 128, :], rw_, r=[rwb_])

        def stage5(s):
            AR.reset()
            hT, hTb = AR.alloc(8 * L, BF16, "h1T"); hT3 = hT.rearrange("p (k t) -> p k t", k=8)
            ld(hT3, H1T[s].rearrange("(k p) t -> p k t", p=128), w=[hTb])
            rw, rwb = AR.alloc(16 * NEXP, F32, "rw"); rw3 = rw.rearrange("p (a e) -> p a e", a=16)
            ld(rw3, RW[s].rearrange("(a p) e -> p a e", p=128), w=[rwb])
            acc, accb = AR.alloc(16 * 1024, F32, "acc"); acc3 = acc.rearrange("p (a d) -> p a d", a=16)
            accbs = [Buf("acc%d" % i) for i in range(16)]
            S.op("pool", lambda e: e.memset(acc, 0.0), w=accbs)
            wsl = []
            for i in range(2):
                g_, gb_ = AR.alloc(8 * 256, BF16, "wg%d" % i); u_, _ = AR.alloc(8 * 256, BF16, "wu%d" % i); d_, _ = AR.alloc(2 * 1024, BF16, "wd%d" % i)
                wsl.append((g_.rearrange("p (k f) -> p k f", k=8), u_.rearrange("p (k f) -> p k f", k=8), d_.rearrange("p (k d) -> p k d", k=2), gb_))
            wr = Rot(wsl)
            sg = Rot([AR.alloc(512, F32, "sg%d" % i) for i in range(2)])
            hid = Rot([AR.alloc(2 * 512, BF16, "hid%d" % i) for i in range(2)])
            pg = Rot([(PS[0], PS[1]), (PS[2], PS[3])]); pd = Rot(PS[4:8])
            for e_ in range(NEXP):
                g3, u3, d3, wb_ = wr.next()
                S.dma("pool", lambda e, g3=g3, e_=e_: e.dma_start(out=g3, in_=ewg[e_].rearrange("(k p) f -> p k f", p=128)), w=[wb_])
                S.dma("pool", lambda e, u3=u3, e_=e_: e.dma_start(out=u3, in_=ewu[e_].rearrange("(k p) f -> p k f", p=128)), w=[wb_])
                S.dma("pool", lambda e, d3=d3, e_=e_: e.dma_start(out=d3, in_=ewd[e_].rearrange("(k p) d -> p k d", p=128)), w=[wb_])
                for tb in range(4):
                    hd, hdb = hid.next()
                    hd3 = hd.rearrange("p (f t) -> p f t", f=2)
                    for fh in range(2):
                        (pG, pGb), (pU, pUb) = pg.next()
                        for k in range(8):
                            mm(pG, g3[:, k, fh * 128:(fh + 1) * 128], hT3[:, k, tb * 512:(tb + 1) * 512], k == 0, k == 7, r=[wb_, hTb], w=[pGb])
                        for k in range(8):
                            mm(pU, u3[:, k, fh * 128:(fh + 1) * 128], hT3[:, k, tb * 512:(tb + 1) * 512], k == 0, k == 7, r=[wb_, hTb], w=[pUb])
                        sg_, sgb = sg.next()
                        act(sg_, pG, AF.Silu, r=[pGb], w=[sgb])
                        tt("dve", hd3[:, fh, :], sg_, pU, ALU.mult, r=[sgb, pUb], w=[hdb])
                    for q in range(4):
                        t_ = tb * 4 + q
                        for dh in range(2):
                            p, pb = pd.next()
                            for fk in range(2):
                                mm(p, hd3[:, fk, q * 128:(q + 1) * 128], d3[:, fk, dh * 512:(dh + 1) * 512], fk == 0, fk == 1, r=[hdb, wb_], w=[pb])
                            stt("dve", acc3[:, t_, dh * 512:(dh + 1) * 512], p, rw3[:, t_, e_:e_ + 1], acc3[:, t_, dh * 512:(dh + 1) * 512],
                                ALU.mult, ALU.add, r=[pb, rwb, accbs[t_]], w=[accbs[t_]])
            rows, rowsb = AR.alloc(2048, F32, "rows2")
            ld(rows[:, 0:1024], rowb("ln2g"), w=[rowsb]); ld(rows[:, 1024:2048], rowb("ln2b"), w=[rowsb])
            hr = Rot([AR.alloc(1024, F32, "hr%d" % i) for i in range(2)])
            oo = Rot([AR.alloc(1024, F32, "oo%d" % i) for i in range(2)])
            sq, sqb = AR.alloc(1024, F32, "sq2"); st8, st8b = AR.alloc(8, F32, "st8b")
            for t_ in range(16):
                h_, hb_ = hr.next()
                ld(h_, H1[s, t_ * 128:(t_ + 1) * 128, :], w=[hb_])
                stt("dve", h_, h_, ALPHA, acc3[:, t_, :], ALU.mult, ALU.add, r=[hb_, accbs[t_]], w=[hb_])
                o_, ob_ = oo.next()
                layer_norm_tile(h_, hb_, rows[:, 0:1024], rows[:, 1024:2048], rowsb, o_, ob_, st8, st8b, sq, sqb)
                stor(out[s, t_ * 128:(t_ + 1) * 128, :], o_, r=[ob_])


        moe = {}

        def stage5_sparse_blocks():
            AR.reset()
            I32_ = mybir.dt.int32
            rw8, rw8b = AR.alloc(32 * 8, F32, "rw8"); rw83 = rw8.rearrange("p (a j) -> p a j", a=32)
            d8i, d8ib = AR.alloc(32 * 8, I32_, "d8i"); d8i3 = d8i.rearrange("p (a j) -> p a j", a=32)
            moe["rw8"] = (rw83, rw8b); moe["d8i"] = (d8i3, d8ib); moe["keep"] = AR.off
            rwall, rwb = AR.alloc(32 * NEXP, F32, "rwall"); rw3 = rwall.rearrange("p (a e) -> p a e", a=32)
            for s_ in range(NSEQ):
                ld(rw3[:, s_ * 16:(s_ + 1) * 16, :], RW[s_].rearrange("(a p) e -> p a e", p=128), w=[rwb])
            su, sub_ = AR.alloc(128, F32, "strictu"); on, _ = AR.alloc(128, F32, "ones")
            ld(su, strictu_d, w=[sub_]); ld(on, ones_d, w=[sub_])
            bst, bstb = AR.alloc(192, F32, "bstart"); pc, _ = AR.alloc(1, F32, "pcol")
            ld(bst, bstart_d.partition_broadcast(128), w=[bstb]); ld(pc, pcol_d, w=[bstb])
            tok_t, tokb = AR.alloc(64, I32_, "tokid")
            ld(tok_t, tokid_d, w=[tokb])
            sel, selb_ = AR.alloc(32 * 64, F32, "sel"); sel3_ = sel.rearrange("p (a e) -> p a e", a=32)
            S.op("dve", lambda e: e.tensor_scalar(out=sel3_, in0=rw3[:, :, 0:64], scalar1=0.0, scalar2=None, op0=ALU.is_gt), r=[rwb], w=[selb_])
            pos, posb = AR.alloc(32 * 64, F32, "pos"); pos3 = pos.rearrange("p (a e) -> p a e", a=32)
            carry, carb = AR.alloc(64, F32, "carry")
            S.op("dve", lambda e: e.memset(carry, 0.0), w=[carb])
            psr = Rot(PS[0:4])
            for i in range(32):
                p, pb = psr.next()
                mm(p[:, 0:64], su, sel3_[:, i, :], True, True, r=[sub_, selb_], w=[pb])
                mm(p[:, 64:128], on, sel3_[:, i, :], True, True, r=[sub_, selb_], w=[pb])
                tt("dve", pos3[:, i, :], p[:, 0:64], carry, ALU.add, r=[pb, carb], w=[posb])
                tt("dve", carry, p[:, 64:128], carry, ALU.add, r=[pb, carb], w=[carb])
            ci, cib = AR.alloc(64, I32_, "cnt_i")
            cp("dve", ci, carry, r=[carb], w=[cib])
            ts("dve", ci, ci, 255, None, ALU.add, None, r=[cib], w=[cib])
            ts("dve", ci, ci, 8, None, ALU.arith_shift_right, None, r=[cib], w=[cib])
            ts("dve", ci, ci, 8, None, ALU.arith_shift_left, None, r=[cib], w=[cib])
            padded, padb = AR.alloc(64, F32, "padded")
            cp("dve", padded, ci, r=[cib], w=[padb])
            pe0, pe0b = AR.alloc(64, F32, "pe0"); pe1, pe1b = AR.alloc(64, F32, "pe1")
            cp("dve", pe0, padded, r=[padb], w=[pe0b])
            cur, curb, oth, othb = pe0, pe0b, pe1, pe1b
            for sh in (1, 2, 4, 8, 16, 32):
                cp("dve", oth[:, 0:sh], cur[:, 0:sh], r=[curb], w=[othb])
                tt("dve", oth[:, sh:64], cur[:, sh:64], cur[:, 0:64 - sh], ALU.add, r=[curb, othb], w=[othb])
                cur, curb, oth, othb = oth, othb, cur, curb
            pend, pendb = cur, curb
            pstart, pstb = oth, othb
            tt("dve", pstart, pend, padded, ALU.subtract, r=[pendb, padb, pstb], w=[pstb])
            key, keyb = AR.alloc(32 * 64, F32, "key"); key3 = key.rearrange("p (a e) -> p a e", a=32)
            tt("dve", key3, pos3, pstart[:, None, :].to_broadcast([128, 32, 64]), ALU.add, r=[posb, pstb], w=[keyb])
            ts("dve", key, key, 1.0, None, ALU.add, None, r=[keyb], w=[keyb])
            tt("dve", key, key, sel, ALU.mult, r=[keyb, selb_], w=[keyb])
            d8, d8b = AR.alloc(32 * 8, F32, "d8"); d83 = d8.rearrange("p (a j) -> p a j", a=32)
            oh, ohb = AR.alloc(8 * 64, F32, "onehot"); oh3 = oh.rearrange("p (j e) -> p j e", j=8)
            for i in range(32):
                S.op("dve", lambda e, i=i: e.max(out=d83[:, i, :], in_=key3[:, i, :]), r=[keyb, d8b], w=[d8b])
                tt("dve", oh3, key3[:, i:i + 1, :].to_broadcast([128, 8, 64]), d83[:, i, :].unsqueeze(2).to_broadcast([128, 8, 64]),
                   ALU.is_equal, r=[keyb, d8b, ohb], w=[ohb])
                tt("dve", oh3, oh3, rw3[:, i:i + 1, 0:64].to_broadcast([128, 8, 64]), ALU.mult, r=[ohb, rwb], w=[ohb])
                S.op("dve", lambda e, i=i: e.tensor_reduce(out=rw83[:, i, :], in_=oh3, axis=AX.X, op=ALU.add), r=[ohb, rw8b], w=[rw8b])
            ts("dve", d8, d8, -1.0, None, ALU.add, None, r=[d8b], w=[d8b])
            cp("dve", d8i, d8, r=[d8b], w=[d8ib])
            fill, fillb = AR.alloc(768, I32_, "fill")
            S.op("pool", lambda e: e.memset(fill, NTOK), w=[fillb])
            slotB = Buf("slot_tok")
            S.dma("sp", lambda e: e.dma_start(out=SLOT_TOK.rearrange("(p c) o -> p (c o)", p=128), in_=fill), r=[fillb], w=[slotB])
            for i in range(32):
                for j in range(8):
                    def _scat(e, i=i, j=j):
                        try:
                            return e.indirect_dma_start(
                                out=SLOT_TOK[:, :], out_offset=bass.IndirectOffsetOnAxis(ap=d8i3[:, i, j:j + 1], axis=0),
                                in_=tok_t[:, 2 * i:2 * i + 2], in_offset=None)
                        except Exception:
                            print("SCATTER FAIL at", i, j, d8i3[:, i, j:j + 1].shape, tok_t[:, 2 * i:2 * i + 2].shape, flush=True)
                            raise
                    S.dma("pool", _scat, r=[d8ib, tokb], wm=[slotB])
            cmp3, cmpb = AR.alloc(192 * 64, F32, "cmp3"); cmp33 = cmp3.rearrange("p (b e) -> p b e", b=192)
            tt("dve", cmp33, pend[:, None, :].to_broadcast([128, 192, 64]), bst.unsqueeze(2).to_broadcast([128, 192, 64]), ALU.is_le,
               r=[pendb, bstb], w=[cmpb])
            be_, beb = AR.alloc(192, F32, "be")
            S.op("dve", lambda e: e.tensor_reduce(out=be_, in_=cmp33, axis=AX.X, op=ALU.add), r=[cmpb], w=[beb])
            ts("dve", be_, be_, 63.0, 128.0, ALU.min, ALU.mult, r=[beb], w=[beb])
            ts("dve", be_, be_, pc[:, 0:1], None, ALU.add, None, r=[beb, bstb], w=[beb])
            widx, widxb = AR.alloc(192, I32_, "widx")
            cp("dve", widx, be_, r=[beb], w=[widxb])
            idb, idbb = AR.alloc(128, BF16, "identb")
            ld(idb, identb_d, w=[idbb])
            ixall, ixallb = AR.alloc(NBLK * 4, I32_, "ixall")
            for b in range(NBLK):
                for hf in range(2):
                    S.dma("sp", lambda e, b=b, hf=hf: e.dma_start(out=ixall[:, b * 4 + 2 * hf: b * 4 + 2 * hf + 2],
                          in_=SLOT_TOK[b * 256 + hf * 128: b * 256 + (hf + 1) * 128, :]), r=[slotB], wm=[ixallb])
            ixtoks = []
            wsl = Rot([(AR.alloc(2048, BF16, "wg%d" % i), AR.alloc(2048, BF16, "wu%d" % i), AR.alloc(2048, BF16, "wd%d" % i)) for i in range(4)])
            xgs = Rot([AR.alloc(2 * 1024, BF16, "xg%d" % i) for i in range(4)])
            xTs = Rot([AR.alloc(8 * 256, BF16, "xT%d" % i) for i in range(2)])
            sgs = Rot([AR.alloc(256, F32, "sg%d" % i) for i in range(2)])
            hids = Rot([AR.alloc(2 * 256, BF16, "hid%d" % i) for i in range(2)])
            yos = Rot([AR.alloc(1024, F32, "yo%d" % i) for i in range(6)])
            ptr = Rot(PS[0:2]); pgu = Rot([(PS[2], PS[3]), (PS[4], PS[5])]); pdn = Rot(PS[6:8])
            yslotB = Buf("yslot")
            blk = {}

            def blk_gather(b):
                ix, ixb = ixall[:, b * 4:(b + 1) * 4], ixallb
                (wg, wgb), (wu, wub), (wd, wdb) = wsl.next()
                for (wt_, wtb_, src_) in ((wg, wgb, EWG2), (wu, wub, EWU2), (wd, wdb, EWD2)):
                    S.dma("pool", lambda e, wt_=wt_, src_=src_, b=b: e.indirect_dma_start(
                        out=wt_, out_offset=None, in_=src_[:, :], in_offset=bass.IndirectOffsetOnAxis(ap=widx[:, b:b + 1], axis=0)),
                        r=[widxb], w=[wtb_])
                xg, xgb = xgs.next()
                for hf in range(2):
                    S.dma("pool", lambda e, xg=xg, ix=ix, hf=hf: e.indirect_dma_start(
                        out=xg[:, hf * 1024:(hf + 1) * 1024], out_offset=None, in_=H1B[:, :],
                        in_offset=bass.IndirectOffsetOnAxis(ap=ix[:, 2 * hf:2 * hf + 1], axis=0)),
                        r=[ixb], w=[xgb])
                blk[b] = {"w": (wg, wgb, wu, wub, wd, wdb), "xg": (xg, xgb)}

            def blk_trans(b):
                xg, xgb = blk[b]["xg"]
                xT, xTb = xTs.next()
                xT3 = xT.rearrange("p (k t) -> p k t", k=8)
                for hf in range(2):
                    for k4 in range(2):
                        p, pb = ptr.next()
                        pbv = p.bitcast(BF16)
                        for q in range(4):
                            k = k4 * 4 + q
                            tr(pbv[:, q * 128:(q + 1) * 128], xg[:, hf * 1024 + k * 128: hf * 1024 + (k + 1) * 128], idb, r=[xgb, idbb], w=[pb])
                        S.op("act", lambda e, pbv=pbv, k4=k4, hf=hf, xT3=xT3: e.activation(
                            out=xT3[:, k4 * 4:(k4 + 1) * 4, hf * 128:(hf + 1) * 128], in_=pbv[:, 0:512].rearrange("p (a b) -> p a b", a=4), func=AF.Copy),
                            r=[pb], w=[xTb])
                blk[b]["xT"] = (xT3, xTb)

            def blk_gu(b):
                wg, wgb, wu, wub, wd, wdb = blk[b]["w"]
                xT3, xTb = blk[b]["xT"]
                g3 = wg.rearrange("p (k f) -> p k f", k=8); u3 = wu.rearrange("p (k f) -> p k f", k=8)
                hd, hdb = hids.next()
                hd3 = hd.rearrange("p (f t) -> p f t", f=2)
                for fh in range(2):
                    (pG, pGb), (pU, pUb) = pgu.next()
                    for k in range(8):
                        mm(pG[:, 0:256], g3[:, k, fh * 128:(fh + 1) * 128], xT3[:, k, :], k == 0, k == 7, r=[wgb, xTb], w=[pGb])
                    for k in range(8):
                        mm(pU[:, 0:256], u3[:, k, fh * 128:(fh + 1) * 128], xT3[:, k, :], k == 0, k == 7, r=[wub, xTb], w=[pUb])
                    sg_, sgb = sgs.next()
                    act(sg_, pG[:, 0:256], AF.Silu, r=[pGb], w=[sgb])
                    tt("dve", hd3[:, fh, :], sg_, pU[:, 0:256], ALU.mult, r=[sgb, pUb], w=[hdb])
                blk[b]["hid"] = (hd3, hdb)

            def blk_down(b):
                wg, wgb, wu, wub, wd, wdb = blk[b]["w"]
                d3 = wd.rearrange("p (k d) -> p k d", k=2)
                hd3, hdb = blk[b]["hid"]
                for hf in range(2):
                    yo, yob = yos.next()
                    for dh in range(2):
                        p, pb = pdn.next()
                        for fk in range(2):
                            mm(p, hd3[:, fk, hf * 128:(hf + 1) * 128], d3[:, fk, dh * 512:(dh + 1) * 512], fk == 0, fk == 1, r=[hdb, wdb], w=[pb])
                        if dh == 0:
                            cp("act", yo[:, 0:512], p, r=[pb], w=[yob])
                        else:
                            cp("dve", yo[:, 512:1024], p, r=[pb], w=[yob])
                    S.dma("sp", lambda e, yo=yo, b=b, hf=hf: e.dma_start(out=YSLOT[b * 256 + hf * 128: b * 256 + (hf + 1) * 128, :], in_=yo),
                          r=[yob], wm=[yslotB])
                del blk[b]

            blk_gather(0)
            blk_gather(1)
            blk_gather(2)
            blk_trans(0)
            for b in range(NBLK):
                blk_gu(b)
                if b + 1 < NBLK:
                    blk_trans(b + 1)
                if b + 3 < NBLK:
                    blk_gather(b + 3)
                blk_down(b)
            moe["yslotB"] = yslotB

        def stage5_sparse_finish(s):
            AR.off = moe["keep"]
            rw83, rw8b = moe["rw8"]; d8i3, d8ib = moe["d8i"]; yslotB = moe["yslotB"]
            hT, hTb = AR.alloc(8 * L, BF16, "h1T"); hT3 = hT.rearrange("p (k t) -> p k t", k=8)
            ld(hT3, H1T[s].rearrange("(k p) t -> p k t", p=128), w=[hTb])
            acc, accb = AR.alloc(16 * 1024, F32, "acc"); acc3 = acc.rearrange("p (a d) -> p a d", a=16)
            accbs = [Buf("acc%d" % i) for i in range(16)]
            g_, gb_ = AR.alloc(8 * 256, BF16, "swg"); u_, _ = AR.alloc(8 * 256, BF16, "swu"); d_, _ = AR.alloc(2 * 1024, BF16, "swd")
            g3 = g_.rearrange("p (k f) -> p k f", k=8); u3 = u_.rearrange("p (k f) -> p k f", k=8); d3 = d_.rearrange("p (k d) -> p k d", k=2)
            if s == 0:
                S.dma("pool", lambda e: e.dma_start(out=g3, in_=ewg[64].rearrange("(k p) f -> p k f", p=128)), w=[gb_])
                S.dma("pool", lambda e: e.dma_start(out=u3, in_=ewu[64].rearrange("(k p) f -> p k f", p=128)), w=[gb_])
                S.dma("pool", lambda e: e.dma_start(out=d3, in_=ewd[64].rearrange("(k p) d -> p k d", p=128)), w=[gb_])
                moe["shw"] = gb_
            else:
                gb_ = moe["shw"]
            sg = Rot([AR.alloc(512, F32, "sg%d" % i) for i in range(2)])
            hid = Rot([AR.alloc(2 * 512, BF16, "hid%d" % i) for i in range(2)])
            pg = Rot([(PS[0], PS[1]), (PS[2], PS[3])]); pd = Rot(PS[4:8])
            for tb in range(4):
                hd, hdb = hid.next()
                hd3 = hd.rearrange("p (f t) -> p f t", f=2)
                for fh in range(2):
                    (pG, pGb), (pU, pUb) = pg.next()
                    for k in range(8):
                        mm(pG, g3[:, k, fh * 128:(fh + 1) * 128], hT3[:, k, tb * 512:(tb + 1) * 512], k == 0, k == 7, r=[gb_, hTb], w=[pGb])
                    for k in range(8):
                        mm(pU, u3[:, k, fh * 128:(fh + 1) * 128], hT3[:, k, tb * 512:(tb + 1) * 512], k == 0, k == 7, r=[gb_, hTb], w=[pUb])
                    sg_, sgb = sg.next()
                    act(sg_, pG, AF.Silu, r=[pGb], w=[sgb])
                    tt("dve", hd3[:, fh, :], sg_, pU, ALU.mult, r=[sgb, pUb], w=[hdb])
                for q in range(4):
                    t_ = tb * 4 + q
                    for dh in range(2):
                        p, pb = pd.next()
                        for fk in range(2):
                            mm(p, hd3[:, fk, q * 128:(q + 1) * 128], d3[:, fk, dh * 512:(dh + 1) * 512], fk == 0, fk == 1, r=[hdb, gb_], w=[pb])
                        cp("act", acc3[:, t_, dh * 512:(dh + 1) * 512], p, r=[pb], w=[accbs[t_]])
            ygs = Rot([AR.alloc(1024, F32, "yg%d" % i) for i in range(8)])
            for t_ in range(16):
                i = s * 16 + t_
                for j in range(8):
                    yg, ygb = ygs.next()
                    S.dma("pool", lambda e, yg=yg, i=i, j=j: e.indirect_dma_start(
                        out=yg, out_offset=None, in_=YSLOT[:, :], in_offset=bass.IndirectOffsetOnAxis(ap=d8i3[:, i, j:j + 1], axis=0)), r=[d8ib, yslotB], w=[ygb])
                    stt("dve", acc3[:, t_, :], yg, rw83[:, i, j:j + 1], acc3[:, t_, :], ALU.mult, ALU.add, r=[ygb, rw8b, accbs[t_]], w=[accbs[t_]])
            rows, rowsb = AR.alloc(2048, F32, "rows2")
            ld(rows[:, 0:1024], rowb("ln2g"), w=[rowsb]); ld(rows[:, 1024:2048], rowb("ln2b"), w=[rowsb])
            hr = Rot([AR.alloc(1024, F32, "hr%d" % i) for i in range(2)])
            oo = Rot([AR.alloc(1024, F32, "oo%d" % i) for i in range(2)])
            sq, sqb = AR.alloc(1024, F32, "sq2"); st8, st8b = AR.alloc(8, F32, "st8b")
            for t_ in range(16):
                h_, hb_ = hr.next()
                ld(h_, H1[s, t_ * 128:(t_ + 1) * 128, :], w=[hb_])
                stt("dve", h_, h_, ALPHA, acc3[:, t_, :], ALU.mult, ALU.add, r=[hb_, accbs[t_]], w=[hb_])
                o_, ob_ = oo.next()
                layer_norm_tile(h_, hb_, rows[:, 0:1024], rows[:, 1024:2048], rowsb, o_, ob_, st8, st8b, sq, sqb)
                stor(out[s, t_ * 128:(t_ + 1) * 128, :], o_, r=[ob_])

        nstage = dbg.get("_nstage", 99) if dbg else 99
        stage0()
        S.barrier()
        for s in range(NSEQ if nstage >= 1 else 0):
            stage1(s)
            S.barrier()
        for s in range(NSEQ if nstage >= 2 else 0):
            for cb in range(2):
                hyena_conv(s, cb, 0, HY[s, 2048 + cb * 512: 2048 + (cb + 1) * 512, :], HY[s, cb * 512:(cb + 1) * 512, :],
                           ZS[s, cb * 512:(cb + 1) * 512, :], F32)
                S.barrier()
                hyena_conv(s, cb, 1, ZS[s, cb * 512:(cb + 1) * 512, :], HY[s, 1024 + cb * 512: 1024 + (cb + 1) * 512, :],
                           YH[s, cb * 512:(cb + 1) * 512, :], BF16)
                S.barrier()
        for s in range(NSEQ if nstage >= 3 else 0):
            stage3(s)
            S.barrier()
        for s in range(NSEQ if nstage >= 4 else 0):
            stage4(s)
            S.barrier()
        if nstage >= 5:
            if SPARSE_MOE:
                stage5_sparse_blocks()
                S.barrier()
                for s in range(NSEQ):
                    stage5_sparse_finish(s)
                    S.barrier()
            else:
                for s in range(NSEQ):
                    stage5(s)
                    S.barrier()
        S.emit()
    return nc


_CONST = None


def _constants():
    global _CONST
    if _CONST is not None:
        return _CONST
    bf = ml_dtypes.bfloat16
    a = np.arange(L, dtype=np.int64)
    prod = np.outer(a, a) % 4096
    ang = prod.astype(np.float64) * (2.0 * np.pi / 4096.0)
    Cm = np.cos(ang).astype(np.float32)
    Sm = np.sin(ang).astype(np.float32)
    tile_ = lambda M: np.ascontiguousarray(M.reshape(16, 128, L).transpose(1, 0, 2)).astype(bf)
    c = {"dftc": tile_(Cm), "dfts": tile_(Sm)}
    t = np.linspace(0.0, 1.0, L, dtype=np.float32)[:, None]
    bands = 16
    w = (2.0 * np.pi * np.arange(L, dtype=np.float32)[:, None] / L).astype(np.float32)
    f = np.linspace(1e-4, bands - 1, bands, dtype=np.float32)[None, :]
    z = np.concatenate([t, np.cos(f * w), -np.sin(f * w)], axis=-1).astype(np.float32)
    c["zT"] = np.ascontiguousarray(z.T)
    max_decay = math.log(1e-2) / 0.3
    min_decay = math.log(1e-2) / 1.5
    deltas = np.linspace(min_decay, max_decay, 1024, dtype=np.float32)[None, :]
    c["decay"] = np.exp(-t * np.abs(deltas)).astype(np.float32)
    alt = np.where(np.arange(L) % 2 == 0, 1.0, -1.0).astype(np.float32)
    c["altcol"] = alt[:128, None].astype(bf)
    c["altrow"] = alt[None, :].astype(bf)
    wsc = np.full((128, 2), 2.0 / 4096.0, np.float32)
    wsc[0, 0] = 1.0 / 4096.0
    c["wsc"] = wsc
    c["identf"] = np.eye(128, dtype=np.float32)
    c["identb"] = np.eye(128, dtype=np.float32).astype(bf)
    s_ = np.arange(128)[:, None]; l_ = np.arange(128)[None, :]
    c["maskf"] = (l_ >= s_).astype(np.float32)
    c["maskb"] = (l_ <= s_).astype(np.float32)
    c["ones"] = np.ones((128, 128), np.float32)
    sel = np.zeros((32, 32, 128), np.float32)
    for h in range(32):
        sel[h, h, :] = 1.0
    c["selc"] = sel.reshape(32, 4096)
    c["tokid"] = np.repeat((np.arange(32)[None, :] * 128 + np.arange(128)[:, None]).astype(np.int32), 2, axis=1)
    c["bstart"] = (np.arange(192, dtype=np.float32) * 256.0)[None, :]
    c["pcol"] = np.arange(128, dtype=np.float32)[:, None]
    c["strictu"] = (l_ > s_).astype(np.float32)
    _CONST = c
    return c


def _pack_params(p):
    f32 = np.float32
    b_in = p["b_in"][0]
    col = np.zeros((128, NCOL), f32)
    hcw, hcb = p["hy_conv_w"][0], p["hy_conv_b"][0]
    for j in range(24):
        sl = slice(j * 128, (j + 1) * 128)
        col[:, CP_HY + 5 * j] = b_in[sl]
        for k in range(3):
            col[:, CP_HY + 5 * j + 1 + k] = hcw[k, sl]
        col[:, CP_HY + 5 * j + 4] = hcb[sl]
    scw, scb = p["ssd_conv_w"][0], p["ssd_conv_b"][0]
    for j in range(32):
        sl = slice(j * 128, (j + 1) * 128)
        col[:, CP_XBC + 7 * j] = b_in[5120 + j * 128: 5120 + (j + 1) * 128]
        for k in range(5):
            col[:, CP_XBC + 7 * j + 1 + k] = scw[k, sl]
        col[:, CP_XBC + 7 * j + 6] = scb[sl]
    for j in range(16):
        col[:, CP_GATE + j] = b_in[9280 + j * 128: 9280 + (j + 1) * 128]
    hb = p["hy_bias"][0]
    for o in range(2):
        for ct in range(8):
            col[:, CP_HYB + o * 8 + ct] = hb[o, ct * 128:(ct + 1) * 128]
    row = np.zeros((1, NROW), f32)

    def put(name, v):
        o, l = RP[name]
        row[0, o:o + l] = np.asarray(v, f32).reshape(-1)
    put("bz", b_in[3072:5120]); put("bdt", b_in[9216:9280]); put("dtb", p["ssd_dt_bias"][0]); put("alog", p["ssd_a_log"][0])
    put("dskip", np.repeat(p["ssd_d"][0], 64)); put("normw", p["ssd_norm_w"][0]); put("bout", p["b_out"][0])
    put("ln1g", p["ln1_g"][0]); put("ln1b", p["ln1_b"][0]); put("ln2g", p["ln2_g"][0]); put("ln2b", p["ln2_b"][0])
    put("rbias", p["router_bias"][0])
    fcols = np.stack([p["hy_f_b1"][0], p["hy_f_freq1"][0], p["hy_f_b2"][0], p["hy_f_freq2"][0], p["hy_f_b3"][0], p["hy_f_freq3"][0]], axis=1).astype(f32)
    d = {
        "w_in": np.ascontiguousarray(p["w_in"][0]), "colpack": col, "rowpack": row,
        "fw1": np.ascontiguousarray(p["hy_f_w1"][0]), "fw2": np.ascontiguousarray(p["hy_f_w2"][0]),
        "fw3": np.ascontiguousarray(p["hy_f_w3"][0]), "fw4": np.ascontiguousarray(p["hy_f_w4"][0]), "fcols": np.ascontiguousarray(fcols),
        "w_hyb": np.ascontiguousarray(p["w_hy_branch"][0]), "w_ssdb": np.ascontiguousarray(p["w_ssd_branch"][0]),
        "w_out": np.ascontiguousarray(p["w_out"][0]), "router_w": np.ascontiguousarray(p["router_w"][0]),
        "ewg": np.concatenate([p["exp_w_gate"][0], p["sh_w_gate"]], axis=0),
        "ewu": np.concatenate([p["exp_w_up"][0], p["sh_w_up"]], axis=0),
        "ewd": np.concatenate([p["exp_w_down"][0], p["sh_w_down"]], axis=0),
    }
    return d


_NC_CACHE = {}


def kernel(**inputs):
    p = {k: np.asarray(v) for k, v in inputs.items()}
    x = np.ascontiguousarray(p["x"], dtype=np.float32)
    shared = dict(_constants())
    shared.update(_pack_params(p))
    n = 8
    in_maps = []
    for c in range(n):
        xs = x[c * NSEQ:(c + 1) * NSEQ]
        m = dict(shared)
        m["x"] = np.ascontiguousarray(xs)
        m["xT"] = np.ascontiguousarray(xs.transpose(0, 2, 1))
        in_maps.append(m)
    if "nc" not in _NC_CACHE:
        _NC_CACHE["nc"] = build_program()
    nc = _NC_CACHE["nc"]
    res = run_bass_kernel_spmd(nc, in_maps, core_ids=list(range(n)))
    outs = [np.asarray(r["out"]) for r in res.results]
    return np.concatenate(outs, axis=0).astype(np.float32)
```

```python
import math
from contextlib import ExitStack
import numpy as np
import ml_dtypes
import concourse.bass as bass
import concourse.mybir as mybir
from concourse.bass_utils import run_bass_kernel_spmd

F32 = mybir.dt.float32
BF16 = mybir.dt.bfloat16
ALU = mybir.AluOpType
AF = mybir.ActivationFunctionType
AX = mybir.AxisListType

ENGS = ("pe", "act", "dve", "pool", "sp")
N_DMA_SEMS = 24
L = 2048
NSEQ = 2
ALPHA = 2.0 ** 0.25
PI = float(np.pi)


class Buf:
    __slots__ = ("name", "w", "r", "x", "mw")

    def __init__(self, name="", x=False):
        self.name = name
        self.w = None
        self.r = []
        self.x = x
        self.mw = []


class Sched:
    def __init__(self, nc, stack):
        self.nc = nc
        self.ops = {e: [] for e in ENGS}
        self.sem = {e: stack.enter_context(nc.semaphore("s_" + e)) for e in ENGS}
        self.dsem = [stack.enter_context(nc.semaphore("d%d" % i)) for i in range(N_DMA_SEMS)]
        self.dcount = [0] * N_DMA_SEMS
        self.dpool = {"sp": list(range(0, 16)), "pool": list(range(16, N_DMA_SEMS))}
        self.dnext = {"sp": 0, "pool": 0}
        self.bar_sem = stack.enter_context(nc.semaphore("bar"))
        self.nbar = 0

    def _deps(self, r, w, eng=None):
        deps = []
        for b in r:
            if b.w is not None:
                deps.append(b.w)
            deps.extend(b.mw)
            if b.x:
                deps.extend(t for t in b.r if not (t[0] == "e" and t[1] == eng))
        for b in w:
            if b.w is not None:
                deps.append(b.w)
            deps.extend(b.r)
        return deps

    def _commit(self, tok, r, w):
        for b in r:
            b.r.append(tok)
        for b in w:
            b.w = tok
            b.r = []

    def op(self, eng, fn, r=(), w=()):
        deps = self._deps(r, w, eng)
        idx = len(self.ops[eng])
        self.ops[eng].append({"fn": fn, "deps": deps, "kind": "c", "needed": False})
        tok = ("e", eng, idx)
        self._commit(tok, r, w)
        return tok

    def dma(self, eng, fn, r=(), w=(), wm=()):
        deps = self._deps(r, w)
        for b in wm:
            if b.w is not None:
                deps.append(b.w)
        pool_ = self.dpool[eng]
        j = pool_[self.dnext[eng] % len(pool_)]
        self.dnext[eng] += 1
        if self.dcount[j] > 0:
            deps.append(("d", j, self.dcount[j] * 16))
        self.dcount[j] += 1
        tok = ("d", j, self.dcount[j] * 16)
        self.ops[eng].append({"fn": fn, "deps": deps, "kind": "d", "dsem": j, "needed": False})
        self._commit(tok, r, w)
        for b in wm:
            b.mw.append(tok)
        return tok

    def barrier(self):
        last = []
        for e in ENGS:
            for i in range(len(self.ops[e]) - 1, -1, -1):
                if self.ops[e][i]["kind"] == "c":
                    last.append(("e", e, i))
                    break
        dl = [("d", j, self.dcount[j] * 16) for j in range(N_DMA_SEMS) if self.dcount[j] > 0]
        self.nbar += 1
        for e in ENGS:
            self.ops[e].append({"fn": None, "deps": last + dl, "kind": "b", "needed": False, "bar": self.nbar})

    def emit(self):
        nc = self.nc
        ops = self.ops
        for e in ENGS:
            for o in ops[e]:
                for d in o["deps"]:
                    if d[0] == "e":
                        ops[d[1]][d[2]]["needed"] = True
        val = {}
        for e in ENGS:
            c = 0
            for i, o in enumerate(ops[e]):
                if o["kind"] == "c" and o["needed"]:
                    c += 1
                    val[(e, i)] = c
        sem, dsem, bar_sem = self.sem, self.dsem, self.bar_sem

        def run(ename, eng):
            waited = {}
            for o in ops[ename]:
                need = {}
                for d in o["deps"]:
                    if d[0] == "e":
                        if d[1] == ename and (ename == "pe" or o["kind"] == "b"):
                            continue
                        key = ("e", d[1])
                        v = val[(d[1], d[2])]
                    else:
                        key = ("d", d[1])
                        v = d[2]
                    if v > need.get(key, 0):
                        need[key] = v
                for key, v in need.items():
                    if waited.get(key, 0) >= v:
                        continue
                    waited[key] = v
                    eng.wait_ge(sem[key[1]] if key[0] == "e" else dsem[key[1]], v)
                if o["kind"] == "b":
                    eng.sem_inc(bar_sem, 1)
                    eng.wait_ge(bar_sem, o["bar"] * len(ENGS))
                    continue
                ins = o["fn"](eng)
                if o["kind"] == "d":
                    ins.then_inc(dsem[o["dsem"]], 16)
                elif o["needed"]:
                    ins.then_inc(sem[ename], 1)

        with nc.Block() as block:
            @block.tensor
            def _(e):
                run("pe", e)

            @block.scalar
            def _(e):
                run("act", e)

            @block.vector
            def _(e):
                run("dve", e)

            @block.gpsimd
            def _(e):
                run("pool", e)

            @block.sync
            def _(e):
                run("sp", e)


class Arena:
    def __init__(self, ap, ncols):
        self.ap = ap
        self.n = ncols
        self.off = 0

    def reset(self):
        self.off = 0

    def alloc(self, nelem, dt=F32, name=""):
        nb = nelem * (2 if dt == BF16 else 4)
        cols = (nb + 3) // 4
        assert self.off + cols <= self.n, ("arena overflow", name, self.off, cols, self.n)
        v = self.ap[:, self.off:self.off + cols]
        self.off += cols
        if dt == BF16:
            v = v.bitcast(BF16)[:, 0:nelem]
        elif dt != F32:
            v = v.bitcast(dt)
        return v, Buf(name)


class Rot:
    def __init__(self, items):
        self.items = items
        self.i = 0

    def next(self):
        it = self.items[self.i % len(self.items)]
        self.i += 1
        return it


CP_HY = 0
CP_XBC = 120
CP_GATE = 344
CP_HYB = 360
NCOL = 376
RP = {}
_o = 0
for _n, _l in [("bz", 2048), ("bdt", 64), ("dtb", 64), ("alog", 64), ("dskip", 2048), ("normw", 2048),
               ("bout", 1024), ("ln1g", 1024), ("ln1b", 1024), ("ln2g", 1024), ("ln2b", 1024), ("rbias", 64)]:
    RP[_n] = (_o, _l)
    _o += _l
NROW = _o
NEXP = 65
SPARSE_MOE = True


def build_program(dbg=None):
    nc = bass.Bass("TRN2", target_bir_lowering=False)

    def din(name, shape, dt=F32):
        return nc.dram_tensor(name, list(shape), dt, kind="ExternalInput").ap()

    def dscr(name, shape, dt=F32):
        kind = "ExternalOutput" if (dbg and name in dbg) else "Internal"
        return nc.dram_tensor(name, list(shape), dt, kind=kind).ap()

    xT = din("xT", [NSEQ, 1024, L])
    x = din("x", [NSEQ, L, 1024])
    w_in = din("w_in", [1024, 11328])
    colpack = din("colpack", [128, NCOL])
    rowpack = din("rowpack", [1, NROW])
    fw1 = din("fw1", [33, 64]); fw2 = din("fw2", [64, 64]); fw3 = din("fw3", [64, 64]); fw4 = din("fw4", [64, 4096])
    fcols = din("fcols", [64, 6])
    w_hyb = din("w_hyb", [1024, 1024]); w_ssdb = din("w_ssdb", [2048, 1024]); w_out = din("w_out", [1024, 1024])
    router_w = din("router_w", [1024, 64])
    ewg = din("ewg", [NEXP, 1024, 256]); ewu = din("ewu", [NEXP, 1024, 256]); ewd = din("ewd", [NEXP, 256, 1024])
    dftc = din("dftc", [128, 16, L], BF16); dfts = din("dfts", [128, 16, L], BF16)
    zT = din("zT", [33, L]); decay = din("decay", [L, 1024])
    altcol = din("altcol", [128, 1], BF16); altrow = din("altrow", [1, L], BF16)
    wsc = din("wsc", [128, 2])
    identf_d = din("identf", [128, 128]); identb_d = din("identb", [128, 128], BF16)
    maskf_d = din("maskf", [128, 128]); maskb_d = din("maskb", [128, 128]); ones_d = din("ones", [128, 128])
    selc_d = din("selc", [32, 4096])
    out = nc.dram_tensor("out", [NSEQ, L, 1024], F32, kind="ExternalOutput").ap()

    KS = dscr("KS", [2, 2, L, 1024]); KN = dscr("KN", [2, 1024])
    HY = dscr("HY", [NSEQ, 3072, L]); SZ = dscr("SZ", [NSEQ, L, 2048]); XBC = dscr("XBC", [NSEQ, 4096, L])
    DTT = dscr("DTT", [NSEQ, L, 64]); G = dscr("G", [NSEQ, 2048, L])
    ZS = dscr("ZS", [NSEQ, 1024, L]); YH = dscr("YH", [NSEQ, 1024, L], BF16); YS = dscr("YS", [NSEQ, 2048, L], BF16)
    CUMF = dscr("CUMF", [NSEQ, 2, 8, 16, 4, 128])
    I32 = mybir.dt.int32
    NTOK = NSEQ * L
    NBLK = 192
    NSLOT = NBLK * 256
    H1B = dscr("H1B", [NTOK + 1, 1024], BF16)
    EWG2 = dscr("EWG2", [64 * 128, 2048], BF16); EWU2 = dscr("EWU2", [64 * 128, 2048], BF16); EWD2 = dscr("EWD2", [64 * 128, 2048], BF16)
    SLOT_TOK = dscr("SLOT_TOK", [NSLOT, 2], I32)
    YSLOT = dscr("YSLOT", [NSLOT, 1024])
    tokid_d = din("tokid", [128, 64], I32); bstart_d = din("bstart", [1, 192]); pcol_d = din("pcol", [128, 1]); strictu_d = din("strictu", [128, 128])
    H1 = dscr("H1", [NSEQ, L, 1024]); H1T = dscr("H1T", [NSEQ, 1024, L], BF16); RW = dscr("RW", [NSEQ, L, NEXP])

    with ExitStack() as st:
        S = Sched(nc, st)
        ACOLS = 53000
        arena_t = st.enter_context(nc.sbuf_tensor("arena", [128, ACOLS], F32))
        AR = Arena(arena_t[:], ACOLS)
        PS = []
        for i in range(8):
            t = st.enter_context(nc.psum_tensor("ps%d" % i, [128, 512], F32))
            PS.append((t[:], Buf("ps%d" % i, x=True)))

        def mm(o, lhsT, rhs, start, stop, r, w):
            S.op("pe", lambda e: e.matmul(o, lhsT, rhs, start=start, stop=stop), r=r, w=w)

        def tr(o, in_, ident, r, w):
            S.op("pe", lambda e: e.transpose(o, in_, ident), r=r, w=w)

        def act(o, in_, func, r, w, bias=0.0, scale=1.0, accum=None):
            if accum is None:
                S.op("act", lambda e: e.activation(out=o, in_=in_, func=func, bias=bias, scale=scale), r=r, w=w)
            else:
                S.op("act", lambda e: e.activation(out=o, in_=in_, func=func, bias=bias, scale=scale, accum_out=accum), r=r, w=w)

        def tt(eng, o, a, b, op, r, w):
            S.op(eng, lambda e: e.tensor_tensor(out=o, in0=a, in1=b, op=op), r=r, w=w)

        def ts(eng, o, a, s1, s2, op0, op1, r, w):
            if op1 is None:
                S.op(eng, lambda e: e.tensor_scalar(out=o, in0=a, scalar1=s1, scalar2=None, op0=op0), r=r, w=w)
            else:
                S.op(eng, lambda e: e.tensor_scalar(out=o, in0=a, scalar1=s1, scalar2=s2, op0=op0, op1=op1), r=r, w=w)

        def stt(eng, o, a, sc, b, op0, op1, r, w, tmp=None):
            if eng == "pool":
                tmp_ap, tmp_b = tmp
                S.op("pool", lambda e: e.tensor_scalar(out=tmp_ap, in0=a, scalar1=sc, scalar2=None, op0=op0), r=list(r) + [tmp_b], w=[tmp_b])
                S.op("pool", lambda e: e.tensor_tensor(out=o, in0=tmp_ap, in1=b, op=op1), r=list(r) + [tmp_b], w=w)
            else:
                S.op(eng, lambda e: e.scalar_tensor_tensor(out=o, in0=a, scalar=sc, in1=b, op0=op0, op1=op1), r=r, w=w)

        def cp(eng, o, a, r, w):
            if eng == "act":
                S.op(eng, lambda e: e.activation(out=o, in_=a, func=AF.Copy), r=r, w=w)
            else:
                S.op(eng, lambda e: e.tensor_copy(out=o, in_=a), r=r, w=w)

        def zero(o, b):
            S.op("dve", lambda e: e.memset(o, 0.0), r=[b], w=[b])

        def ld(o, in_, w, r=(), q="sp"):
            S.dma(q, lambda e: e.dma_start(out=o, in_=in_), r=r, w=w)

        def stor(o, in_, r, q="sp"):
            S.dma(q, lambda e: e.dma_start(out=o, in_=in_), r=r, w=())

        def rowb(name):
            o, l = RP[name]
            return rowpack[:, o:o + l].partition_broadcast(128)

        def wrap_sin(pre, tmp, o, rb, nparts, ncols):
            for _ in range(2):
                ts("dve", tmp, pre, -PI, 2 * PI, ALU.is_lt, ALU.mult, r=rb, w=rb)
                tt("dve", pre, pre, tmp, ALU.add, r=rb, w=rb)
                ts("dve", tmp, pre, PI, -2 * PI, ALU.is_gt, ALU.mult, r=rb, w=rb)
                tt("dve", pre, pre, tmp, ALU.add, r=rb, w=rb)
            ts("dve", pre, pre, -PI, PI, ALU.max, ALU.min, r=rb, w=rb)
            act(o, pre, AF.Sin, r=rb, w=rb)

        def dft_block_loader():
            slots = []
            for i in range(2):
                c, cb_ = AR.alloc(16 * 512, BF16, "dftc%d" % i)
                s_, _ = AR.alloc(16 * 512, BF16, "dfts%d" % i)
                slots.append((c.rearrange("p (a b) -> p a b", a=16), s_.rearrange("p (a b) -> p a b", a=16), cb_))
            resident = [None, None]
            lru = [0, 1]

            def load(idx):
                if idx in resident:
                    k = resident.index(idx)
                else:
                    k = lru[0]
                    c, s_, b = slots[k]
                    ld(c, dftc[:, :, idx * 512:(idx + 1) * 512], w=[b])
                    ld(s_, dfts[:, :, idx * 512:(idx + 1) * 512], w=[b])
                    resident[k] = idx
                lru.remove(k)
                lru.append(k)
                return slots[k]
            return load

        def stage0():
            AR.reset()
            zt, zb = AR.alloc(L, F32, "zt")
            w1, wb = AR.alloc(64, F32, "w1"); w2, _ = AR.alloc(64, F32); w3, _ = AR.alloc(64, F32)
            w4, _ = AR.alloc(4096, F32); fc, _ = AR.alloc(6, F32); fb, fbb = AR.alloc(3, F32, "fb")
            hA, hAb = AR.alloc(L, F32, "hA"); hB, hBb = AR.alloc(L, F32, "hB")
            pre, preb = AR.alloc(512, F32, "pre"); tmp, _ = AR.alloc(512, F32)
            ac, acb = AR.alloc(1, BF16, "altc")
            wc, wcb = AR.alloc(2, F32, "wsc")
            ld(zt[:33], zT, w=[zb]); ld(w1[:33], fw1, w=[wb]); ld(w2[:64], fw2, w=[wb]); ld(w3[:64], fw3, w=[wb])
            ld(w4[:64], fw4, w=[wb]); ld(fc[:64], fcols, w=[wb]); ld(ac, altcol, w=[acb]); ld(wc, wsc, w=[wcb])
            for i in range(3):
                tt("dve", fb[:64, i:i + 1], fc[:64, 2 * i:2 * i + 1], fc[:64, 2 * i + 1:2 * i + 2], ALU.mult, r=[wb], w=[fbb])
            psr = Rot(PS[0:2])
            chain = [(w1, 33, zt, zb, hA, hAb), (w2, 64, hA, hAb, hB, hBb), (w3, 64, hB, hBb, hA, hAb)]
            for i, (w, K, hin, hinb, hout, houtb) in enumerate(chain):
                for tb in range(4):
                    p, pb = psr.next()
                    mm(p[:64, :], w[:K, 0:64], hin[:K, tb * 512:(tb + 1) * 512], True, True, r=[wb, hinb], w=[pb])
                    act(pre[:64], p[:64, :], AF.Identity, r=[pb, wb, fbb], w=[preb],
                        bias=fb[:64, i:i + 1], scale=fc[:64, 2 * i + 1:2 * i + 2])
                    wrap_sin(pre[:64], tmp[:64], hout[:64, tb * 512:(tb + 1) * 512], [preb, houtb], 64, 512)
            h3, h3b = hA, hAb
            dec, decb = AR.alloc(16 * 512, F32, "dec")
            dec3 = dec.rearrange("p (a b) -> p a b", a=16)
            Aa, Ab = AR.alloc(16 * 512, BF16, "A"); Bm, Bb = AR.alloc(16 * 512, BF16, "Bm")
            A3 = Aa.rearrange("p (a b) -> p a b", a=16); B3 = Bm.rearrange("p (a b) -> p a b", a=16)
            kf, kfb = AR.alloc(512, F32, "kf"); kb_, kbb = AR.alloc(512, F32, "kb")
            kst = [AR.alloc(1024, F32, "kst%d" % i) for i in range(2)]
            kstr = Rot(kst)
            kn, knb = AR.alloc(512, F32, "kn")
            load = dft_block_loader()
            for cb in range(2):
                ld(dec3, decay[:, cb * 512:(cb + 1) * 512].rearrange("(a p) c -> p a c", p=128), w=[decb])
                for o in range(2):
                    colf = o * 2048 + cb * 512
                    colb = o * 2048 + 1024 + cb * 512
                    for t_ in range(16):
                        p0, p0b = PS[2]; p1, p1b = PS[3]
                        mm(p0, h3[:64, t_ * 128:(t_ + 1) * 128], w4[:64, colf:colf + 512], True, True, r=[h3b, wb], w=[p0b])
                        mm(p1, h3[:64, t_ * 128:(t_ + 1) * 128], w4[:64, colb:colb + 512], True, True, r=[h3b, wb], w=[p1b])
                        tt("dve", kf, p0, dec3[:, t_, :], ALU.mult, r=[p0b, decb], w=[kfb])
                        tt("dve", kb_, p1, dec3[:, t_, :], ALU.mult, r=[p1b, decb], w=[kbb])
                        tt("pool", A3[:, t_, :], kf, kb_, ALU.add, r=[kfb, kbb], w=[Ab])
                        tt("pool", B3[:, t_, :], kb_, kf, ALU.subtract, r=[kfb, kbb], w=[Bb])
                    for fbk in (range(4) if (cb * 2 + o) % 2 == 0 else (3, 2, 1, 0)):
                        cblk, sblk, dbf = load(fbk)
                        for j in range(4):
                            ft = fbk * 4 + j
                            pR, pRb = PS[4 + (ft % 2)]; pI, pIb = PS[6 + (ft % 2)]
                            for d_ in range(16):
                                mm(pR, cblk[:, d_, j * 128:(j + 1) * 128], A3[:, d_, :], d_ == 0, d_ == 15, r=[dbf, Ab], w=[pRb])
                            for d_ in range(16):
                                mm(pI, sblk[:, d_, j * 128:(j + 1) * 128], B3[:, d_, :], d_ == 0, d_ == 15, r=[dbf, Bb], w=[pIb])
                            k_, k_b = kstr.next()
                            wcol = wc[:, 0:1] if ft == 0 else wc[:, 1:2]
                            act(k_[:, 0:512], pR, AF.Copy, r=[pRb, wcb], w=[k_b], scale=wcol)
                            act(k_[:, 512:1024], pI, AF.Copy, r=[pIb, wcb], w=[k_b], scale=wcol)
                            stor(KS[o, 0, ft * 128:(ft + 1) * 128, cb * 512:(cb + 1) * 512], k_[:, 0:512], r=[k_b])
                            stor(KS[o, 1, ft * 128:(ft + 1) * 128, cb * 512:(cb + 1) * 512], k_[:, 512:1024], r=[k_b])
                    pN, pNb = PS[0]
                    for d_ in range(16):
                        mm(pN[0:1, :], ac[:, 0:1], A3[:, d_, :], d_ == 0, d_ == 15, r=[acb, Ab], w=[pNb])
                    act(kn[0:1, :], pN[0:1, :], AF.Copy, r=[pNb], w=[knb], scale=1.0 / 4096.0)
                    stor(KN[o:o + 1, cb * 512:(cb + 1) * 512], kn[0:1, :], r=[knb])

        def stage1(s):
            AR.reset()
            xtb, xtbb = AR.alloc(8 * L, BF16, "xTb")
            xt3 = xtb.rearrange("p (k t) -> p k t", k=8)
            S.dma("pool", lambda e: e.dma_start(out=xt3, in_=xT[s].rearrange("(k p) t -> p k t", p=128)), w=[xtbb])
            cpk, cpb = AR.alloc(NCOL, F32, "colpack")
            ld(cpk, colpack, w=[cpb])
            wslots = []
            for i in range(2):
                w_, wb_ = AR.alloc(8 * 512, BF16, "wch%d" % i)
                wslots.append((w_.rearrange("p (k c) -> p k c", k=8), wb_))
            wrot = Rot(wslots)
            pbufs = Rot([AR.alloc(L + 8, F32, "P%d" % i) for i in range(2)])
            obufs = Rot([AR.alloc(L, F32, "O%d" % i) for i in range(3)])
            psr = Rot(PS[0:4])
            pst = Rot(PS[4:7])
            ctmp = AR.alloc(L, F32, "convtmp")

            def load_w(col0, n):
                w3, wb_ = wrot.next()
                S.dma("pool", lambda e: e.dma_start(out=w3[:, :, 0:n], in_=w_in.rearrange("(k p) c -> p k c", p=128)[:, :, col0:col0 + n]), w=[wb_])
                return w3, wb_

            def fm_tile(w3, wb_, ct, bias_col, func, P, Pb, poff):
                for tb in range(4):
                    p, pb = psr.next()
                    for k in range(8):
                        mm(p, w3[:, k, ct * 128:(ct + 1) * 128], xt3[:, k, tb * 512:(tb + 1) * 512], k == 0, k == 7, r=[wb_, xtbb], w=[pb])
                    act(P[:, poff + tb * 512: poff + (tb + 1) * 512], p, func, r=[pb, cpb], w=[Pb], bias=bias_col)

            for ch in range(6):
                w3, wb_ = load_w(ch * 512, 512)
                for ct in range(4):
                    j = ch * 4 + ct
                    c0 = CP_HY + 5 * j
                    P, Pb = pbufs.next()
                    S.op("pool", lambda e, P=P: e.memset(P[:, 0:4], 0.0), w=[Pb])
                    S.op("pool", lambda e, P=P: e.memset(P[:, L + 4:L + 8], 0.0), w=[Pb])
                    fm_tile(w3, wb_, ct, cpk[:, c0:c0 + 1], AF.Identity, P, Pb, 4)
                    O, Ob = obufs.next()
                    eng = "dve"
                    ts(eng, O, P[:, 3:3 + L], cpk[:, c0 + 1:c0 + 2], cpk[:, c0 + 4:c0 + 5], ALU.mult, ALU.add, r=[Pb, cpb], w=[Ob])
                    stt(eng, O, P[:, 4:4 + L], cpk[:, c0 + 2:c0 + 3], O, ALU.mult, ALU.add, r=[Pb, cpb, Ob], w=[Ob], tmp=ctmp)
                    stt(eng, O, P[:, 5:5 + L], cpk[:, c0 + 3:c0 + 4], O, ALU.mult, ALU.add, r=[Pb, cpb, Ob], w=[Ob], tmp=ctmp)
                    stor(HY[s, j * 128:(j + 1) * 128, :], O, r=[Ob])
            for ch in range(8):
                w3, wb_ = load_w(5120 + ch * 512, 512)
                for ct in range(4):
                    j = ch * 4 + ct
                    c0 = CP_XBC + 7 * j
                    P, Pb = pbufs.next()
                    S.op("pool", lambda e, P=P: e.memset(P[:, 0:4], 0.0), w=[Pb])
                    S.op("pool", lambda e, P=P: e.memset(P[:, L + 4:L + 8], 0.0), w=[Pb])
                    fm_tile(w3, wb_, ct, cpk[:, c0:c0 + 1], AF.Identity, P, Pb, 4)
                    O, Ob = obufs.next()
                    eng = "dve"
                    ts(eng, O, P[:, 2:2 + L], cpk[:, c0 + 1:c0 + 2], cpk[:, c0 + 6:c0 + 7], ALU.mult, ALU.add, r=[Pb, cpb], w=[Ob])
                    for k in range(1, 5):
                        stt(eng, O, P[:, 2 + k:2 + k + L], cpk[:, c0 + 1 + k:c0 + 2 + k], O, ALU.mult, ALU.add, r=[Pb, cpb, Ob], w=[Ob], tmp=ctmp)
                    act(O, O, AF.Silu, r=[Ob], w=[Ob])
                    stor(XBC[s, j * 128:(j + 1) * 128, :], O, r=[Ob])
            for ch in range(4):
                w3, wb_ = load_w(9280 + ch * 512, 512)
                for ct in range(4):
                    j = ch * 4 + ct
                    O, Ob = obufs.next()
                    fm_tile(w3, wb_, ct, cpk[:, CP_GATE + j:CP_GATE + j + 1], AF.Sigmoid, O, Ob, 0)
                    stor(G[s, j * 128:(j + 1) * 128, :], O, r=[Ob])
            bz, bzb = AR.alloc(2048, F32, "bz")
            ld(bz, rowb("bz"), w=[bzb])
            zo = Rot([AR.alloc(512, F32, "zo%d" % i) for i in range(3)])
            for ch in range(4):
                w3, wb_ = load_w(3072 + ch * 512, 512)
                for t_ in range(16):
                    p, pb = pst.next()
                    for k in range(8):
                        mm(p, xt3[:, k, t_ * 128:(t_ + 1) * 128], w3[:, k, :], k == 0, k == 7, r=[wb_, xtbb], w=[pb])
                    O, Ob = zo.next()
                    tt("dve", O, p, bz[:, ch * 512:(ch + 1) * 512], ALU.add, r=[pb, bzb], w=[Ob])
                    act(O, O, AF.Silu, r=[Ob], w=[Ob])
                    stor(SZ[s, t_ * 128:(t_ + 1) * 128, ch * 512:(ch + 1) * 512], O, r=[Ob])
            bd, bdb = AR.alloc(64, F32, "bd"); bd2, _ = AR.alloc(64, F32)
            ld(bd, rowb("bdt"), w=[bdb]); ld(bd2, rowb("dtb"), w=[bdb])
            tt("dve", bd, bd, bd2, ALU.add, r=[bdb], w=[bdb])
            w3, wb_ = load_w(9216, 64)
            dto, dtob = AR.alloc(16 * 64, F32, "dto")
            dto3 = dto.rearrange("p (a b) -> p a b", a=16)
            for t_ in range(16):
                p, pb = pst.next()
                for k in range(8):
                    mm(p[:, 0:64], xt3[:, k, t_ * 128:(t_ + 1) * 128], w3[:, k, 0:64], k == 0, k == 7, r=[wb_, xtbb], w=[pb])
                tt("dve", dto3[:, t_, :], p[:, 0:64], bd, ALU.add, r=[pb, bdb], w=[dtob])
            act(dto, dto, AF.Exp, r=[dtob], w=[dtob])
            act(dto, dto, AF.Ln, r=[dtob], w=[dtob], bias=1.0)
            stor(DTT[s].rearrange("(a p) c -> p a c", p=128), dto3, r=[dtob])

        def hyena_conv(s, cb, o, src, gate, dst, dst_dt):
            AR.reset()
            cpk, cpb = AR.alloc(NCOL, F32, "colpack")
            ld(cpk, colpack, w=[cpb])
            idb, idbb = AR.alloc(128, BF16, "identb")
            ld(idb, identb_d, w=[idbb])
            ac, acb = AR.alloc(1, BF16, "altc"); arw, arwb = AR.alloc(L, BF16, "altrow")
            ld(ac, altcol, w=[acb]); ld(arw[0:1, :], altrow, w=[arwb])
            knr, knrb = AR.alloc(512, F32, "knr")
            ld(knr[0:1, :], KN[o:o + 1, cb * 512:(cb + 1) * 512], w=[knrb])
            uT, uTb = AR.alloc(16 * 512, BF16, "uT")
            uT3 = uT.rearrange("p (a b) -> p a b", a=16)
            Yr, Yrb = AR.alloc(16 * 512, BF16, "Yr"); Zi, Zib = AR.alloc(16 * 512, BF16, "Zi")
            Yr3 = Yr.rearrange("p (a b) -> p a b", a=16); Zi3 = Zi.rearrange("p (a b) -> p a b", a=16)
            yn, ynb = AR.alloc(512, BF16, "yn")
            load = dft_block_loader()
            kslots = Rot([AR.alloc(2 * 4 * 512, F32, "K%d" % i) for i in range(2)])
            srcs = Rot([AR.alloc(L, F32, "src%d" % i) for i in range(2)])
            sbf, sbfb = AR.alloc(L, BF16, "srcbf")
            t1, t1b = AR.alloc(512, F32, "t1"); t2, t2b = AR.alloc(512, F32, "t2")
            t3, t3b = AR.alloc(512, F32, "t3"); t4, t4b = AR.alloc(512, F32, "t4")
            ep = Rot([(AR.alloc(512, F32, "es%d" % i), AR.alloc(512, F32, "eg%d" % i), AR.alloc(512, dst_dt, "eo%d" % i)) for i in range(2)])
            ptr = Rot(PS[0:2])
            for ct in range(4):
                sr, srb = srcs.next()
                ld(sr, src[ct * 128:(ct + 1) * 128, :], w=[srb])
                cp("dve", sbf, sr, r=[srb], w=[sbfb])
                for g4 in range(4):
                    p, pb = ptr.next()
                    pbv = p.bitcast(BF16)
                    for q in range(4):
                        t_ = g4 * 4 + q
                        tr(pbv[:, q * 128:(q + 1) * 128], sbf[:, t_ * 128:(t_ + 1) * 128], idb, r=[sbfb, idbb], w=[pb])
                    S.op("act", lambda e, g4=g4, ct=ct, pbv=pbv: e.activation(
                        out=uT3[:, g4 * 4:(g4 + 1) * 4, ct * 128:(ct + 1) * 128],
                        in_=pbv[:, 0:512].rearrange("p (a b) -> p a b", a=4), func=AF.Copy), r=[pb], w=[uTb])
            for fbk in range(4):
                cblk, sblk, dbf = load(fbk)
                (kk, kkb) = kslots.next()
                kk4 = kk.rearrange("p (r j c) -> p r j c", r=2, j=4)
                for ri in range(2):
                    ld(kk4[:, ri], KS[o, ri, fbk * 512:(fbk + 1) * 512, cb * 512:(cb + 1) * 512].rearrange("(j p) c -> p j c", p=128), w=[kkb])
                for j in range(4):
                    ft = fbk * 4 + j
                    pC, pCb = PS[2 + (ft % 2)]; pS_, pSb = PS[4 + (ft % 2)]
                    for t_ in range(16):
                        mm(pC, cblk[:, t_, j * 128:(j + 1) * 128], uT3[:, t_, :], t_ == 0, t_ == 15, r=[dbf, uTb], w=[pCb])
                    for t_ in range(16):
                        mm(pS_, sblk[:, t_, j * 128:(j + 1) * 128], uT3[:, t_, :], t_ == 0, t_ == 15, r=[dbf, uTb], w=[pSb])
                    kr = kk4[:, 0, j, :]; ki = kk4[:, 1, j, :]
                    tt("dve", t1, pC, kr, ALU.mult, r=[pCb, kkb], w=[t1b])
                    tt("dve", t2, pS_, ki, ALU.mult, r=[pSb, kkb], w=[t2b])
                    tt("pool", Yr3[:, ft, :], t1, t2, ALU.add, r=[t1b, t2b], w=[Yrb])
                    tt("dve", t3, pS_, kr, ALU.mult, r=[pSb, kkb], w=[t3b])
                    tt("dve", t4, pC, ki, ALU.mult, r=[pCb, kkb], w=[t4b])
                    tt("pool", Zi3[:, ft, :], t3, t4, ALU.subtract, r=[t3b, t4b], w=[Zib])
            pN, pNb = PS[6]
            for t_ in range(16):
                mm(pN[0:1, :], ac[:, 0:1], uT3[:, t_, :], t_ == 0, t_ == 15, r=[acb, uTb], w=[pNb])
            tt("dve", yn[0:1, :], pN[0:1, :], knr[0:1, :], ALU.mult, r=[pNb, knrb], w=[ynb])
            pinv = Rot([PS[6], PS[7], PS[0], PS[1]])
            for tb in (3, 2, 1, 0):
                cblk, sblk, dbf = load(tb)
                for ct in range(4):
                    p, pb = pinv.next()
                    for ft in range(16):
                        mm(p, Yr3[:, ft, ct * 128:(ct + 1) * 128], cblk[:, ft, :], ft == 0, False, r=[Yrb, dbf], w=[pb])
                    for ft in range(16):
                        mm(p, Zi3[:, ft, ct * 128:(ct + 1) * 128], sblk[:, ft, :], False, False, r=[Zib, dbf], w=[pb])
                    mm(p, yn[0:1, ct * 128:(ct + 1) * 128], arw[0:1, tb * 512:(tb + 1) * 512], False, True, r=[ynb, arwb], w=[pb])
                    (es, esb), (eg, egb), (eo, eob) = ep.next()
                    ld(es, src[ct * 128:(ct + 1) * 128, tb * 512:(tb + 1) * 512], w=[esb])
                    ld(eg, gate[ct * 128:(ct + 1) * 128, tb * 512:(tb + 1) * 512], w=[egb])
                    bcol = cpk[:, CP_HYB + o * 8 + cb * 4 + ct: CP_HYB + o * 8 + cb * 4 + ct + 1]
                    stt("dve", es, es, bcol, p, ALU.mult, ALU.add, r=[esb, cpb, pb], w=[esb])
                    tt("pool", eo, es, eg, ALU.mult, r=[esb, egb], w=[eob])
                    stor(dst[ct * 128:(ct + 1) * 128, tb * 512:(tb + 1) * 512], eo, r=[eob])

        def stage3(s):
            AR.reset()
            idf, idfb = AR.alloc(128, F32, "identf"); idb, idbb = AR.alloc(128, BF16, "identb")
            mkf, mkb_ = AR.alloc(128, F32, "maskf"); mkb, _ = AR.alloc(128, F32, "maskb"); on, _ = AR.alloc(128, F32, "ones")
            sel, selb = AR.alloc(512, F32, "sel")
            ld(idf, identf_d, w=[idfb]); ld(idb, identb_d, w=[idbb]); ld(mkf, maskf_d, w=[mkb_]); ld(mkb, maskb_d, w=[mkb_])
            ld(on, ones_d, w=[mkb_])
            sel3 = sel.rearrange("p (h m) -> p h m", h=4)
            dtt, dttb = AR.alloc(16 * 64, F32, "dtt"); dtt3 = dtt.rearrange("p (a b) -> p a b", a=16)
            ld(dtt3, DTT[s].rearrange("(a p) c -> p a c", p=128), w=[dttb])
            nA, nAb = AR.alloc(64, F32, "negA")
            ld(nA, rowb("alog"), w=[nAb])
            act(nA, nA, AF.Exp, r=[nAb], w=[nAb])
            ts("dve", nA, nA, -1.0, None, ALU.mult, None, r=[nAb], w=[nAb])
            at_, atb = AR.alloc(16 * 64, F32, "a_tok"); at3 = at_.rearrange("p (a b) -> p a b", a=16)
            tt("dve", at3, dtt3, nA[:, None, :].to_broadcast([128, 16, 64]), ALU.mult, r=[dttb, nAb], w=[atb])
            cum, cumb = AR.alloc(16 * 64, F32, "cum"); cum3 = cum.rearrange("p (a b) -> p a b", a=16)
            tot, totb = AR.alloc(16 * 64, F32, "tot"); tot3 = tot.rearrange("p (a b) -> p a b", a=16)
            cFf, cFfb = AR.alloc(L, F32, "cumFf"); cFb, cFbb = AR.alloc(L, F32, "cumFb")
            psr = Rot(PS[0:4])
            for c in range(16):
                p, pb = psr.next()
                mm(p[:, 0:32], mkf, at3[:, c, 0:32], True, True, r=[mkb_, atb], w=[pb])
                mm(p[:, 32:64], mkb, at3[:, c, 32:64], True, True, r=[mkb_, atb], w=[pb])
                mm(p[:, 64:128], on, at3[:, c, :], True, True, r=[mkb_, atb], w=[pb])
                cp("dve", cum3[:, c, :], p[:, 0:64], r=[pb], w=[cumb])
                cp("dve", tot3[:, c, :], p[:, 64:128], r=[pb], w=[totb])
                p2, p2b = psr.next()
                mm(p2[0:32, 0:128], at3[:, c, 0:32], mkf, True, True, r=[mkb_, atb], w=[p2b])
                mm(p2[0:32, 128:256], at3[:, c, 32:64], mkb, True, True, r=[mkb_, atb], w=[p2b])
                cp("dve", cFf[0:32, c * 128:(c + 1) * 128], p2[0:32, 0:128], r=[p2b], w=[cFfb])
                cp("dve", cFb[0:32, c * 128:(c + 1) * 128], p2[0:32, 128:256], r=[p2b], w=[cFbb])
            cumDB = [[Buf("cumf_dram%d_%d" % (d, g_)) for g_ in range(8)] for d in range(2)]
            for d, (cF_, cF_b) in enumerate([(cFf, cFfb), (cFb, cFbb)]):
                for g_ in range(8):
                    S.dma("sp", lambda e, d=d, g_=g_, cF_=cF_: e.dma_start(
                        out=CUMF[s, d, g_].rearrange("c h l -> h c l"),
                        in_=cF_[g_ * 4:(g_ + 1) * 4, :].rearrange("p (c l) -> p c l", c=16)), r=[cF_b], w=[cumDB[d][g_]])
            ncum, ncumb = AR.alloc(16 * 64, F32, "ncum")
            ts("dve", ncum, cum, -1.0, None, ALU.mult, None, r=[cumb], w=[ncumb])
            ncum3 = ncum.rearrange("p (a b) -> p a b", a=16)
            dte, dteb = AR.alloc(16 * 64, F32, "dte"); dte3 = dte.rearrange("p (a b) -> p a b", a=16)
            tt("dve", dte, tot, cum, ALU.subtract, r=[totb, cumb], w=[dteb])
            act(dte, dte, AF.Exp, r=[dteb], w=[dteb])
            tt("dve", dte, dte, dtt, ALU.mult, r=[dteb, dttb], w=[dteb])
            cdec, cdecb = AR.alloc(16 * 64, F32, "cdec"); cdec3 = cdec.rearrange("p (a b) -> p a b", a=16)
            act(cdec, tot, AF.Exp, r=[totb], w=[cdecb])
            dsk, dskb = AR.alloc(256, F32, "dskip"); nw, nwb = AR.alloc(256, F32, "normw")
            xsF = [AR.alloc(L, F32, "xsF%d" % i) for i in range(2)]
            BTs = [AR.alloc(L, BF16, "BT%d" % i) for i in range(2)]; CTs2 = [AR.alloc(L, BF16, "CT%d" % i) for i in range(2)]
            xtok, xtokb = AR.alloc(16 * 256, F32, "xtok"); xtok3 = xtok.rearrange("p (a b) -> p a b", a=16)
            btok, btokb = AR.alloc(16 * 128, BF16, "btok"); btok3 = btok.rearrange("p (a b) -> p a b", a=16)
            stf = [AR.alloc(16 * 256, F32, "st%d" % d) for d in range(2)]
            prv = [AR.alloc(16 * 256, BF16, "prv%d" % d) for d in range(2)]
            rtmps = [AR.alloc(256, F32, "rtmp%d" % d) for d in range(2)]
            rowbuf = Rot([AR.alloc(512, F32, "rowb%d" % i) for i in range(6)])
            xe = Rot([AR.alloc(256, BF16, "xe%d" % i) for i in range(4)])
            xd = Rot([AR.alloc(256, BF16, "xd%d" % i) for i in range(4)])
            cbm = [[AR.alloc(128, F32, "cbm%d_%d" % (par, d)) for d in range(2)] for par in range(2)]
            dif = Rot([AR.alloc(512, F32, "dif%d" % i) for i in range(2)])
            Dm = Rot([AR.alloc(512, F32, "D%d" % i) for i in range(2)])
            MT = Rot([AR.alloc(512, BF16, "MT%d" % i) for i in range(4)])
            Eb = Rot([AR.alloc(512, F32, "Eb%d" % i) for i in range(2)])
            CTs = Rot([AR.alloc(512, BF16, "CTs%d" % i) for i in range(4)])
            yv = Rot([AR.alloc(256, F32, "y%d" % i) for i in range(2)])
            szr = Rot([AR.alloc(256, F32, "sz%d" % i) for i in range(2)])
            ysq, ysqb = AR.alloc(256, F32, "ysq")
            st4r = Rot([AR.alloc(4, F32, "stat%d" % i) for i in range(2)])
            ybf = Rot([AR.alloc(256, BF16, "ybf%d" % i) for i in range(2)])
            yso = Rot([AR.alloc(256, BF16, "yso%d" % i) for i in range(2)])
            ptr = Rot(PS[0:4] + PS[6:8])
            misc = [PS[4][0], PS[5][0]]
            miscB = [(PS[4][1],) * 3, (PS[5][1],) * 3]
            h4 = lambda ap: ap.rearrange("p (h q) -> p h q", h=4)
            def load_xs(g_):
                for i in range(2):
                    ld(xsF[i][0], XBC[s, g_ * 256 + i * 128: g_ * 256 + (i + 1) * 128, :], w=[xsF[i][1]])

            def load_bc(g_):
                bt_, btb_ = BTs[g_ % 2]; ct_, ctb_ = CTs2[g_ % 2]
                S.dma("pool", lambda e, g_=g_, bt_=bt_: e.dma_start(out=bt_, in_=XBC[s, 2048 + g_ * 128: 2048 + (g_ + 1) * 128, :]), w=[btb_])
                S.dma("pool", lambda e, g_=g_, ct_=ct_: e.dma_start(out=ct_, in_=XBC[s, 3072 + g_ * 128: 3072 + (g_ + 1) * 128, :]), w=[ctb_])

            load_xs(0)
            load_bc(0)
            for g in range(8):
                BT, BTb = BTs[g % 2]; CT, CTb = CTs2[g % 2]
                o_, l_ = RP["dskip"]
                ld(dsk, rowpack[:, o_ + g * 256:o_ + (g + 1) * 256].partition_broadcast(128), w=[dskb])
                o_, l_ = RP["normw"]
                ld(nw, rowpack[:, o_ + g * 256:o_ + (g + 1) * 256].partition_broadcast(128), w=[nwb])
                for c in range(16):
                    p, pb = ptr.next()
                    for i in range(2):
                        tr(p[:, i * 128:(i + 1) * 128], xsF[i][0][:, c * 128:(c + 1) * 128], idf, r=[xsF[i][1], idfb], w=[pb])
                    cp("act", xtok3[:, c, :], p[:, 0:256], r=[pb], w=[xtokb])
                for c4 in range(4):
                    p, pb = ptr.next()
                    pbv = p.bitcast(BF16)
                    for q in range(4):
                        c = c4 * 4 + q
                        tr(pbv[:, q * 128:(q + 1) * 128], BT[:, c * 128:(c + 1) * 128], idb, r=[BTb, idbb], w=[pb])
                    S.op("act", lambda e, c4=c4, pbv=pbv: e.activation(out=btok3[:, c4 * 4:(c4 + 1) * 4, :],
                         in_=pbv[:, 0:512].rearrange("p (a b) -> p a b", a=4), func=AF.Copy), r=[pb], w=[btokb])
                if g + 1 < 8:
                    load_xs(g + 1)
                for c in range(16):
                    for d in range(2):
                        (xe_, xeb) = xe.next()
                        tt("pool" if d == 0 else "dve", h4(xe_), h4(xtok3[:, c, :]),
                           dte3[:, c, d * 32 + g * 4: d * 32 + g * 4 + 4].unsqueeze(2).to_broadcast([128, 4, 64]), ALU.mult,
                           r=[xtokb, dteb], w=[xeb])
                        p, pb = ptr.next()
                        mm(p[:, 0:256], btok3[:, c, :], xe_, True, True, r=[btokb, xeb], w=[pb])
                        cp("act", stf[d][0][:, c * 256:(c + 1) * 256], p[:, 0:256], r=[pb], w=[stf[d][1]])
                for k in range(1, 15):
                    for d in range(2):
                        c = k if d == 0 else 15 - k
                        cprev = c - 1 if d == 0 else c + 1
                        arr, arrb = stf[d]
                        rt, rtb = rtmps[d]
                        tt("dve", h4(rt), h4(arr[:, cprev * 256:(cprev + 1) * 256]),
                           cdec3[:, c, d * 32 + g * 4: d * 32 + g * 4 + 4].unsqueeze(2).to_broadcast([128, 4, 64]), ALU.mult,
                           r=[arrb, cdecb, rtb], w=[rtb])
                        tt("dve", arr[:, c * 256:(c + 1) * 256], rt, arr[:, c * 256:(c + 1) * 256], ALU.add, r=[rtb, arrb], w=[arrb])
                S.op("pool", lambda e: e.memset(prv[0][0][:, 0:256], 0.0), r=[prv[0][1]], w=[prv[0][1]])
                S.op("pool", lambda e: e.memset(prv[1][0][:, 15 * 256:16 * 256], 0.0), r=[prv[1][1]], w=[prv[1][1]])
                cp("act", prv[0][0][:, 256:4096], stf[0][0][:, 0:3840], r=[stf[0][1]], w=[prv[0][1]])
                cp("act", prv[1][0][:, 0:3840], stf[1][0][:, 256:4096], r=[stf[1][1]], w=[prv[1][1]])

                if g + 1 < 8:
                    load_bc(g + 1)
                it = {}

                def front_a(c):
                    par = c % 2
                    pm = misc[par]; mB = miscB[par][0]
                    d_ = {}
                    mm(pm[:, 0:128], BT[:, c * 128:(c + 1) * 128], CT[:, c * 128:(c + 1) * 128], True, True, r=[BTb, CTb], w=[mB])
                    tt("dve", cbm[par][0][0], pm[:, 0:128], mkf, ALU.mult, r=[mB, mkb_], w=[cbm[par][0][1]])
                    tt("dve", cbm[par][1][0], pm[:, 0:128], mkb, ALU.mult, r=[mB, mkb_], w=[cbm[par][1][1]])
                    d_["dirs"] = []
                    for d in range(2):
                        pr, prb = rowbuf.next()
                        ld(pr, CUMF[s, d, g, c:c + 1].rearrange("o h l -> o (h l)").partition_broadcast(128), w=[prb], r=[cumDB[d][g]])
                        df, dfb = dif.next()
                        c0 = d * 32 + g * 4
                        tt("dve", df.rearrange("p (h l) -> p h l", h=4), pr.rearrange("p (h l) -> p h l", h=4),
                           ncum3[:, c, c0:c0 + 4].unsqueeze(2).to_broadcast([128, 4, 128]), ALU.add, r=[prb, ncumb, dfb], w=[dfb])
                        D_, Db = Dm.next()
                        act(D_, df, AF.Exp, r=[dfb], w=[Db])
                        E_, Ebb = Eb.next()
                        act(E_, pr, AF.Exp, r=[prb], w=[Ebb])
                        xd_, xdb = xd.next()
                        tt("pool", h4(xd_), h4(xtok3[:, c, :]),
                           dtt3[:, c, c0:c0 + 4].unsqueeze(2).to_broadcast([128, 4, 64]), ALU.mult, r=[xtokb, dttb], w=[xdb])
                        d_["dirs"].append((D_, Db, E_, Ebb, xd_, xdb, d))
                    y_, yb_ = yv.next()
                    sz, szb = szr.next()
                    ld(sz, SZ[s, c * 128:(c + 1) * 128, g * 256:(g + 1) * 256], w=[szb])
                    tt("pool", y_, xtok3[:, c, :], dsk, ALU.mult, r=[xtokb, dskb], w=[yb_])
                    d_["y"] = (y_, yb_, sz, szb)
                    it[c] = d_

                def front_b(c):
                    par = c % 2
                    d_ = it[c]
                    mts = []
                    for (D_, Db, E_, Ebb, xd_, xdb, d) in d_["dirs"]:
                        mt, mtb = MT.next()
                        stt("dve", mt.rearrange("p (h l) -> p h l", h=4), D_.rearrange("p (h l) -> p h l", h=4), 1.0,
                            cbm[par][d][0][:, None, :].to_broadcast([128, 4, 128]), ALU.min, ALU.mult, r=[Db, cbm[par][d][1]], w=[mtb])
                        cs, csb = CTs.next()
                        tt("pool", cs.rearrange("p (h l) -> p h l", h=4), E_.rearrange("p (h l) -> p h l", h=4),
                           CT[:, None, c * 128:(c + 1) * 128].to_broadcast([128, 4, 128]), ALU.mult, r=[Ebb, CTb], w=[csb])
                        mts.append((mt, mtb, cs, csb, xd_, xdb, d))
                    d_["mts"] = mts

                def back_pe(c):
                    par = c % 2
                    pm = misc[par]; mB = miscB[par][0]
                    yp = pm[:, 128:384]
                    for h in range(4):
                        for k, (mt, mtb, cs, csb, xd_, xdb, d) in enumerate(it[c]["mts"]):
                            mm(yp[:, h * 64:(h + 1) * 64], mt[:, h * 128:(h + 1) * 128], xd_[:, h * 64:(h + 1) * 64], k == 0, False,
                               r=[mtb, xdb], w=[mB])
                            mm(yp[:, h * 64:(h + 1) * 64], cs[:, h * 128:(h + 1) * 128], prv[d][0][:, c * 256 + h * 64: c * 256 + (h + 1) * 64],
                               False, k == 1, r=[csb, prv[d][1]], w=[mB])

                def back_a(c):
                    par = c % 2
                    pm = misc[par]; mB = miscB[par][0]
                    yp = pm[:, 128:384]
                    y_, yb_, sz, szb = it[c]["y"]
                    st4, st4b = st4r.next()
                    tt("dve", y_, y_, yp, ALU.add, r=[yb_, mB], w=[yb_])
                    tt("dve", y_, y_, sz, ALU.mult, r=[yb_, szb], w=[yb_])
                    tt("dve", ysq, y_, y_, ALU.mult, r=[yb_, ysqb], w=[ysqb])
                    S.op("dve", lambda e, st4=st4: e.reduce_sum(out=st4[:, 0:1], in_=ysq, axis=AX.X), r=[ysqb, st4b], w=[st4b])
                    act(st4[:, 1:2], st4[:, 0:1], AF.Ln, r=[st4b], w=[st4b], bias=1e-5, scale=1.0 / 256.0)
                    act(st4[:, 2:3], st4[:, 1:2], AF.Exp, r=[st4b], w=[st4b], scale=-0.5)
                    it[c]["st4"] = (st4, st4b)

                def back_b(c):
                    y_, yb_, sz, szb = it[c]["y"]
                    st4, st4b = it[c]["st4"]
                    yb16, yb16b = ybf.next()
                    stt("dve", yb16, y_, st4[:, 2:3], nw, ALU.mult, ALU.mult, r=[yb_, st4b, nwb], w=[yb16b])
                    it[c]["yb16"] = (yb16, yb16b)

                def back_t(c):
                    yb16, yb16b = it[c]["yb16"]
                    pt, ptb = ptr.next()
                    pbv = pt.bitcast(BF16)
                    for i in range(2):
                        tr(pbv[:, i * 128:(i + 1) * 128], yb16[:, i * 128:(i + 1) * 128], idb, r=[yb16b, idbb], w=[ptb])
                    yo, yob = yso.next()
                    cp("act", yo, pbv[:, 0:256], r=[ptb], w=[yob])
                    stor(YS[s, g * 256:(g + 1) * 256, c * 128:(c + 1) * 128].rearrange("(i p) t -> p i t", p=128),
                         yo.rearrange("p (i t) -> p i t", i=2), r=[yob])
                    del it[c]

                front_a(0)
                front_b(0)
                for c in range(16):
                    if c + 1 < 16:
                        front_a(c + 1)
                    back_pe(c)
                    if c >= 1:
                        back_t(c - 1)
                    back_a(c)
                    if c + 1 < 16:
                        front_b(c + 1)
                    back_b(c)
                back_t(15)

        def layer_norm_tile(r_, rb, g_row, b_row, gb, o_, ob, st4, st4b, sq, sqb):
            S.op("dve", lambda e: e.reduce_sum(out=st4[:, 0:1], in_=r_, axis=AX.X), r=[rb], w=[st4b])
            ts("dve", st4[:, 1:2], st4[:, 0:1], -1.0 / 1024.0, None, ALU.mult, None, r=[st4b], w=[st4b])
            ts("dve", r_, r_, st4[:, 1:2], None, ALU.add, None, r=[rb, st4b], w=[rb])
            tt("dve", sq, r_, r_, ALU.mult, r=[rb, sqb], w=[sqb])
            S.op("dve", lambda e: e.reduce_sum(out=st4[:, 2:3], in_=sq, axis=AX.X), r=[sqb, st4b], w=[st4b])
            act(st4[:, 3:4], st4[:, 2:3], AF.Ln, r=[st4b], w=[st4b], bias=1e-5, scale=1.0 / 1024.0)
            act(st4[:, 4:5], st4[:, 3:4], AF.Exp, r=[st4b], w=[st4b], scale=-0.5)
            stt("dve", o_, r_, st4[:, 4:5], g_row, ALU.mult, ALU.mult, r=[rb, st4b, gb], w=[ob])
            tt("dve", o_, o_, b_row, ALU.add, r=[ob, gb], w=[ob])

        def stage4(s):
            AR.reset()
            whb, whbb = AR.alloc(8 * 1024, BF16, "whb"); wsb, _ = AR.alloc(16 * 1024, BF16, "wsb"); wo, _ = AR.alloc(8 * 1024, BF16, "wo")
            whb3 = whb.rearrange("p (k c) -> p k c", k=8); wsb3 = wsb.rearrange("p (k c) -> p k c", k=16); wo3 = wo.rearrange("p (k c) -> p k c", k=8)
            S.dma("pool", lambda e: e.dma_start(out=whb3, in_=w_hyb.rearrange("(k p) c -> p k c", p=128)), w=[whbb])
            S.dma("pool", lambda e: e.dma_start(out=wsb3, in_=w_ssdb.rearrange("(k p) c -> p k c", p=128)), w=[whbb])
            S.dma("pool", lambda e: e.dma_start(out=wo3, in_=w_out.rearrange("(k p) c -> p k c", p=128)), w=[whbb])
            rwt, rwtb = AR.alloc(8 * 64, F32, "routerw"); rw3 = rwt.rearrange("p (k c) -> p k c", k=8)
            ld(rw3, router_w.rearrange("(k p) c -> p k c", p=128), w=[rwtb])
            idf, idfb = AR.alloc(128, F32, "identf")
            ld(idf, identf_d, w=[idfb])
            rows, rowsb = AR.alloc(3 * 1024 + 64, F32, "rows")
            ld(rows[:, 0:1024], rowb("bout"), w=[rowsb]); ld(rows[:, 1024:2048], rowb("ln1g"), w=[rowsb])
            ld(rows[:, 2048:3072], rowb("ln1b"), w=[rowsb]); ld(rows[:, 3072:3136], rowb("rbias"), w=[rowsb])
            yh, yhb = AR.alloc(8 * 512, BF16, "yh"); ys, ysb = AR.alloc(16 * 512, BF16, "ys")
            yh3 = yh.rearrange("p (k t) -> p k t", k=8); ys3 = ys.rearrange("p (k t) -> p k t", k=16)
            gt = Rot([(AR.alloc(512, F32, "gh%d" % i), AR.alloc(512, F32, "gs%d" % i)) for i in range(2)])
            ta, tab = AR.alloc(512, F32, "ta"); tb_, tbb = AR.alloc(512, F32, "tb")
            mT, mTb = AR.alloc(8 * 512, BF16, "mT"); mT3 = mT.rearrange("p (k t) -> p k t", k=8)
            xr = Rot([AR.alloc(1024, F32, "xr%d" % i) for i in range(2)])
            ho = Rot([AR.alloc(1024, F32, "ho%d" % i) for i in range(2)])
            sq, sqb = AR.alloc(1024, F32, "sq"); st8, st8b = AR.alloc(8, F32, "st8")
            hT32, hT32b = AR.alloc(8 * 128, F32, "hT32"); hT16 = Rot([AR.alloc(8 * 128, BF16, "hT16_%d" % i) for i in range(2)])
            sc, scb = AR.alloc(64, F32, "sc"); bi, bib = AR.alloc(64, F32, "bi"); m8, m8b = AR.alloc(8, F32, "m8")
            gs, gsb = AR.alloc(8, F32, "gs"); gm, gmb = AR.alloc(8, F32, "gm"); pen, penb = AR.alloc(8, F32, "pen")
            msk, mskb = AR.alloc(64, F32, "msk"); rwo = Rot([AR.alloc(NEXP, F32, "rwo%d" % i) for i in range(2)])
            hrow16 = Rot([AR.alloc(1024, BF16, "hrow16_%d" % i) for i in range(2)])
            wcast = Rot([AR.alloc(2048, BF16, "wcast%d" % i) for i in range(8)])
            if s == 0:
                zr, zrb = AR.alloc(1024, BF16, "zerorow")
                S.op("pool", lambda e: e.memset(zr[0:1, :], 0.0), w=[zrb])
                stor(H1B[NTOK:NTOK + 1, :], zr[0:1, :], r=[zrb])
            pbr = Rot(PS[0:4]); pot = Rot(PS[4:6]); ptr = Rot(PS[6:8])
            for tb in range(4):
                ld(yh3, YH[s, :, tb * 512:(tb + 1) * 512].rearrange("(k p) t -> p k t", p=128), w=[yhb])
                ld(ys3, YS[s, :, tb * 512:(tb + 1) * 512].rearrange("(k p) t -> p k t", p=128), w=[ysb])
                for dt_ in range(8):
                    (gh, ghb), (gs_, gsb_) = gt.next()
                    ld(gh, G[s, dt_ * 128:(dt_ + 1) * 128, tb * 512:(tb + 1) * 512], w=[ghb])
                    ld(gs_, G[s, 1024 + dt_ * 128:1024 + (dt_ + 1) * 128, tb * 512:(tb + 1) * 512], w=[gsb_])
                    pH, pHb = pbr.next()
                    for k in range(8):
                        mm(pH, whb3[:, k, dt_ * 128:(dt_ + 1) * 128], yh3[:, k, :], k == 0, k == 7, r=[whbb, yhb], w=[pHb])
                    pS_, pSb = pbr.next()
                    for k in range(16):
                        mm(pS_, wsb3[:, k, dt_ * 128:(dt_ + 1) * 128], ys3[:, k, :], k == 0, k == 15, r=[whbb, ysb], w=[pSb])
                    tt("dve", ta, pH, gh, ALU.mult, r=[pHb, ghb, tab], w=[tab])
                    tt("dve", tb_, pS_, gs_, ALU.mult, r=[pSb, gsb_, tbb], w=[tbb])
                    tt("dve", mT3[:, dt_, :], ta, tb_, ALU.add, r=[tab, tbb], w=[mTb])
                for q in range(4):
                    t_ = tb * 4 + q
                    xr_, xrb = xr.next()
                    ld(xr_, x[s, t_ * 128:(t_ + 1) * 128, :], w=[xrb])
                    for dh in range(2):
                        p, pb = pot.next()
                        for k in range(8):
                            mm(p, mT3[:, k, q * 128:(q + 1) * 128], wo3[:, k, dh * 512:(dh + 1) * 512], k == 0, k == 7, r=[mTb, whbb], w=[pb])
                        stt("dve", xr_[:, dh * 512:(dh + 1) * 512], xr_[:, dh * 512:(dh + 1) * 512], ALPHA, p, ALU.mult, ALU.add, r=[xrb, pb], w=[xrb])
                    tt("dve", xr_, xr_, rows[:, 0:1024], ALU.add, r=[xrb, rowsb], w=[xrb])
                    h_, hb_ = ho.next()
                    layer_norm_tile(xr_, xrb, rows[:, 1024:2048], rows[:, 2048:3072], rowsb, h_, hb_, st8, st8b, sq, sqb)
                    stor(H1[s, t_ * 128:(t_ + 1) * 128, :], h_, r=[hb_])
                    hb16, hb16b = hrow16.next()
                    cp("act", hb16, h_, r=[hb_], w=[hb16b])
                    stor(H1B[s * L + t_ * 128: s * L + (t_ + 1) * 128, :], hb16, r=[hb16b])
                    for e_ in (s * 32 + t_ * 2, s * 32 + t_ * 2 + 1):
                        for (src_, dst_, kk, pat) in ((ewg, EWG2, 8, "(k p) f -> p k f"), (ewu, EWU2, 8, "(k p) f -> p k f"), (ewd, EWD2, 2, "(k p) d -> p k d")):
                            wt, wtb = wcast.next()
                            S.dma("pool", lambda e, wt=wt, src_=src_, e_=e_, kk=kk, pat=pat: e.dma_start(
                                out=wt.rearrange("p (k c) -> p k c", k=kk), in_=src_[e_].rearrange(pat, p=128)), w=[wtb])
                            stor(dst_[e_ * 128:(e_ + 1) * 128, :], wt, r=[wtb])
                    h16, h16b = hT16.next()
                    for k4 in range(2):
                        p, pb = ptr.next()
                        for k in range(4):
                            kk = k4 * 4 + k
                            tr(p[:, k * 128:(k + 1) * 128], h_[:, kk * 128:(kk + 1) * 128], idf, r=[hb_, idfb], w=[pb])
                        cp("act", hT32[:, k4 * 512:(k4 + 1) * 512], p, r=[pb], w=[hT32b])
                        cp("act", h16[:, k4 * 512:(k4 + 1) * 512], p, r=[pb], w=[h16b])
                    stor(H1T[s, :, t_ * 128:(t_ + 1) * 128].rearrange("(k p) t -> p k t", p=128), h16.rearrange("p (k t) -> p k t", k=8), r=[h16b])
                    p, pb = ptr.next()
                    for k in range(8):
                        mm(p[:, 0:64], hT32[:, k * 128:(k + 1) * 128], rw3[:, k, :], k == 0, k == 7, r=[hT32b, rwtb], w=[pb])
                    act(sc, p[:, 0:64], AF.Exp, r=[pb], w=[scb], scale=-1.0)
                    ts("dve", sc, sc, 1.0, None, ALU.add, None, r=[scb], w=[scb])
                    S.op("dve", lambda e: e.reciprocal(out=sc, in_=sc), r=[scb], w=[scb])
                    tt("dve", bi, sc, rows[:, 3072:3136], ALU.add, r=[scb, rowsb], w=[bib])
                    for g in range(8):
                        S.op("dve", lambda e, g=g: e.max(out=m8, in_=bi[:, g * 8:(g + 1) * 8]), r=[bib, m8b], w=[m8b])
                        tt("dve", gs[:, g:g + 1], m8[:, 0:1], m8[:, 1:2], ALU.add, r=[m8b], w=[gsb])
                    S.op("dve", lambda e: e.max(out=m8, in_=gs), r=[gsb, m8b], w=[m8b])
                    ts("dve", gm, gs, m8[:, 3:4], None, ALU.is_ge, None, r=[gsb, m8b], w=[gmb])
                    ts("dve", pen, gm, -1.0, 1e9, ALU.add, ALU.mult, r=[gmb], w=[penb])
                    tt("dve", msk.rearrange("p (g e) -> p g e", g=8), bi.rearrange("p (g e) -> p g e", g=8),
                       gm.unsqueeze(2).to_broadcast([128, 8, 8]), ALU.mult, r=[bib, gmb], w=[mskb])
                    tt("dve", msk.rearrange("p (g e) -> p g e", g=8), msk.rearrange("p (g e) -> p g e", g=8),
                       pen.unsqueeze(2).to_broadcast([128, 8, 8]), ALU.add, r=[mskb, penb], w=[mskb])
                    S.op("dve", lambda e: e.max(out=m8, in_=msk), r=[mskb, m8b], w=[m8b])
                    ts("dve", msk, msk, m8[:, 7:8], None, ALU.is_ge, None, r=[mskb, m8b], w=[mskb])
                    rw_, rwb_ = rwo.next()
                    tt("dve", rw_[:, 0:64], sc, msk, ALU.mult, r=[scb, mskb], w=[rwb_])
                    S.op("dve", lambda e, rw_=rw_: e.reduce_sum(out=st8[:, 5:6], in_=rw_[:, 0:64], axis=AX.X), r=[rwb_], w=[st8b])
                    S.op("dve", lambda e: e.reciprocal(out=st8[:, 6:7], in_=st8[:, 5:6]), r=[st8b], w=[st8b])
                    ts("dve", rw_[:, 0:64], rw_[:, 0:64], st8[:, 6:7], 2.5, ALU.mult, ALU.mult, r=[rwb_, st8b], w=[rwb_])
                    S.op("pool", lambda e, rw_=rw_: e.memset(rw_[:, 64:65], 1.0), r=[rwb_], w=[rwb_])
                    stor(RW[s, t_ * 128:(t_ + 1) * 128, :], rw_, r=[rwb_])

        def stage5(s):
            AR.reset()
            hT, hTb = AR.alloc(8 * L, BF16, "h1T"); hT3 = hT.rearrange("p (k t) -> p k t", k=8)
            ld(hT3, H1T[s].rearrange("(k p) t -> p k t", p=128), w=[hTb])
            rw, rwb = AR.alloc(16 * NEXP, F32, "rw"); rw3 = rw.rearrange("p (a e) -> p a e", a=16)
            ld(rw3, RW[s].rearrange("(a p) e -> p a e", p=128), w=[rwb])
            acc, accb = AR.alloc(16 * 1024, F32, "acc"); acc3 = acc.rearrange("p (a d) -> p a d", a=16)
            accbs = [Buf("acc%d" % i) for i in range(16)]
            S.op("pool", lambda e: e.memset(acc, 0.0), w=accbs)
            wsl = []
            for i in range(2):
                g_, gb_ = AR.alloc(8 * 256, BF16, "wg%d" % i); u_, _ = AR.alloc(8 * 256, BF16, "wu%d" % i); d_, _ = AR.alloc(2 * 1024, BF16, "wd%d" % i)
                wsl.append((g_.rearrange("p (k f) -> p k f", k=8), u_.rearrange("p (k f) -> p k f", k=8), d_.rearrange("p (k d) -> p k d", k=2), gb_))
            wr = Rot(wsl)
            sg = Rot([AR.alloc(512, F32, "sg%d" % i) for i in range(2)])
            hid = Rot([AR.alloc(2 * 512, BF16, "hid%d" % i) for i in range(2)])
            pg = Rot([(PS[0], PS[1]), (PS[2], PS[3])]); pd = Rot(PS[4:8])
            for e_ in range(NEXP):
                g3, u3, d3, wb_ = wr.next()
                S.dma("pool", lambda e, g3=g3, e_=e_: e.dma_start(out=g3, in_=ewg[e_].rearrange("(k p) f -> p k f", p=128)), w=[wb_])
                S.dma("pool", lambda e, u3=u3, e_=e_: e.dma_start(out=u3, in_=ewu[e_].rearrange("(k p) f -> p k f", p=128)), w=[wb_])
                S.dma("pool", lambda e, d3=d3, e_=e_: e.dma_start(out=d3, in_=ewd[e_].rearrange("(k p) d -> p k d", p=128)), w=[wb_])
                for tb in range(4):
                    hd, hdb = hid.next()
                    hd3 = hd.rearrange("p (f t) -> p f t", f=2)
                    for fh in range(2):
                        (pG, pGb), (pU, pUb) = pg.next()
                        for k in range(8):
                            mm(pG, g3[:, k, fh * 128:(fh + 1) * 128], hT3[:, k, tb * 512:(tb + 1) * 512], k == 0, k == 7, r=[wb_, hTb], w=[pGb])
                        for k in range(8):
                            mm(pU, u3[:, k, fh * 128:(fh + 1) * 128], hT3[:, k, tb * 512:(tb + 1) * 512], k == 0, k == 7, r=[wb_, hTb], w=[pUb])
                        sg_, sgb = sg.next()
                        act(sg_, pG, AF.Silu, r=[pGb], w=[sgb])
                        tt("dve", hd3[:, fh, :], sg_, pU, ALU.mult, r=[sgb, pUb], w=[hdb])
                    for q in range(4):
                        t_ = tb * 4 + q
                        for dh in range(2):
                            p, pb = pd.next()
                            for fk in range(2):
                                mm(p, hd3[:, fk, q * 128:(q + 1) * 128], d3[:, fk, dh * 512:(dh + 1) * 512], fk == 0, fk == 1, r=[hdb, wb_], w=[pb])
                            stt("dve", acc3[:, t_, dh * 512:(dh + 1) * 512], p, rw3[:, t_, e_:e_ + 1], acc3[:, t_, dh * 512:(dh + 1) * 512],
                                ALU.mult, ALU.add, r=[pb, rwb, accbs[t_]], w=[accbs[t_]])
            rows, rowsb = AR.alloc(2048, F32, "rows2")
            ld(rows[:, 0:1024], rowb("ln2g"), w=[rowsb]); ld(rows[:, 1024:2048], rowb("ln2b"), w=[rowsb])
            hr = Rot([AR.alloc(1024, F32, "hr%d" % i) for i in range(2)])
            oo = Rot([AR.alloc(1024, F32, "oo%d" % i) for i in range(2)])
            sq, sqb = AR.alloc(1024, F32, "sq2"); st8, st8b = AR.alloc(8, F32, "st8b")
            for t_ in range(16):
                h_, hb_ = hr.next()
                ld(h_, H1[s, t_ * 128:(t_ + 1) * 128, :], w=[hb_])
                stt("dve", h_, h_, ALPHA, acc3[:, t_, :], ALU.mult, ALU.add, r=[hb_, accbs[t_]], w=[hb_])
                o_, ob_ = oo.next()
                layer_norm_tile(h_, hb_, rows[:, 0:1024], rows[:, 1024:2048], rowsb, o_, ob_, st8, st8b, sq, sqb)
                stor(out[s, t_ * 128:(t_ + 1) * 128, :], o_, r=[ob_])


        moe = {}

        def stage5_sparse_blocks():
            AR.reset()
            I32_ = mybir.dt.int32
            rw8, rw8b = AR.alloc(32 * 8, F32, "rw8"); rw83 = rw8.rearrange("p (a j) -> p a j", a=32)
            d8i, d8ib = AR.alloc(32 * 8, I32_, "d8i"); d8i3 = d8i.rearrange("p (a j) -> p a j", a=32)
            moe["rw8"] = (rw83, rw8b); moe["d8i"] = (d8i3, d8ib); moe["keep"] = AR.off
            rwall, rwb = AR.alloc(32 * NEXP, F32, "rwall"); rw3 = rwall.rearrange("p (a e) -> p a e", a=32)
            for s_ in range(NSEQ):
                ld(rw3[:, s_ * 16:(s_ + 1) * 16, :], RW[s_].rearrange("(a p) e -> p a e", p=128), w=[rwb])
            su, sub_ = AR.alloc(128, F32, "strictu"); on, _ = AR.alloc(128, F32, "ones")
            ld(su, strictu_d, w=[sub_]); ld(on, ones_d, w=[sub_])
            bst, bstb = AR.alloc(192, F32, "bstart"); pc, _ = AR.alloc(1, F32, "pcol")
            ld(bst, bstart_d.partition_broadcast(128), w=[bstb]); ld(pc, pcol_d, w=[bstb])
            tok_t, tokb = AR.alloc(64, I32_, "tokid")
            ld(tok_t, tokid_d, w=[tokb])
            sel, selb_ = AR.alloc(32 * 64, F32, "sel"); sel3_ = sel.rearrange("p (a e) -> p a e", a=32)
            S.op("dve", lambda e: e.tensor_scalar(out=sel3_, in0=rw3[:, :, 0:64], scalar1=0.0, scalar2=None, op0=ALU.is_gt), r=[rwb], w=[selb_])
            pos, posb = AR.alloc(32 * 64, F32, "pos"); pos3 = pos.rearrange("p (a e) -> p a e", a=32)
            carry, carb = AR.alloc(64, F32, "carry")
            S.op("dve", lambda e: e.memset(carry, 0.0), w=[carb])
            psr = Rot(PS[0:4])
            for i in range(32):
                p, pb = psr.next()
                mm(p[:, 0:64], su, sel3_[:, i, :], True, True, r=[sub_, selb_], w=[pb])
                mm(p[:, 64:128], on, sel3_[:, i, :], True, True, r=[sub_, selb_], w=[pb])
                tt("dve", pos3[:, i, :], p[:, 0:64], carry, ALU.add, r=[pb, carb], w=[posb])
                tt("dve", carry, p[:, 64:128], carry, ALU.add, r=[pb, carb], w=[carb])
            ci, cib = AR.alloc(64, I32_, "cnt_i")
            cp("dve", ci, carry, r=[carb], w=[cib])
            ts("dve", ci, ci, 255, None, ALU.add, None, r=[cib], w=[cib])
            ts("dve", ci, ci, 8, None, ALU.arith_shift_right, None, r=[cib], w=[cib])
            ts("dve", ci, ci, 8, None, ALU.arith_shift_left, None, r=[cib], w=[cib])
            padded, padb = AR.alloc(64, F32, "padded")
            cp("dve", padded, ci, r=[cib], w=[padb])
            pe0, pe0b = AR.alloc(64, F32, "pe0"); pe1, pe1b = AR.alloc(64, F32, "pe1")
            cp("dve", pe0, padded, r=[padb], w=[pe0b])
            cur, curb, oth, othb = pe0, pe0b, pe1, pe1b
            for sh in (1, 2, 4, 8, 16, 32):
                cp("dve", oth[:, 0:sh], cur[:, 0:sh], r=[curb], w=[othb])
                tt("dve", oth[:, sh:64], cur[:, sh:64], cur[:, 0:64 - sh], ALU.add, r=[curb, othb], w=[othb])
                cur, curb, oth, othb = oth, othb, cur, curb
            pend, pendb = cur, curb
            pstart, pstb = oth, othb
            tt("dve", pstart, pend, padded, ALU.subtract, r=[pendb, padb, pstb], w=[pstb])
            key, keyb = AR.alloc(32 * 64, F32, "key"); key3 = key.rearrange("p (a e) -> p a e", a=32)
            tt("dve", key3, pos3, pstart[:, None, :].to_broadcast([128, 32, 64]), ALU.add, r=[posb, pstb], w=[keyb])
            ts("dve", key, key, 1.0, None, ALU.add, None, r=[keyb], w=[keyb])
            tt("dve", key, key, sel, ALU.mult, r=[keyb, selb_], w=[keyb])
            d8, d8b = AR.alloc(32 * 8, F32, "d8"); d83 = d8.rearrange("p (a j) -> p a j", a=32)
            oh, ohb = AR.alloc(8 * 64, F32, "onehot"); oh3 = oh.rearrange("p (j e) -> p j e", j=8)
            for i in range(32):
                S.op("dve", lambda e, i=i: e.max(out=d83[:, i, :], in_=key3[:, i, :]), r=[keyb, d8b], w=[d8b])
                tt("dve", oh3, key3[:, i:i + 1, :].to_broadcast([128, 8, 64]), d83[:, i, :].unsqueeze(2).to_broadcast([128, 8, 64]),
                   ALU.is_equal, r=[keyb, d8b, ohb], w=[ohb])
                tt("dve", oh3, oh3, rw3[:, i:i + 1, 0:64].to_broadcast([128, 8, 64]), ALU.mult, r=[ohb, rwb], w=[ohb])
                S.op("dve", lambda e, i=i: e.tensor_reduce(out=rw83[:, i, :], in_=oh3, axis=AX.X, op=ALU.add), r=[ohb, rw8b], w=[rw8b])
            ts("dve", d8, d8, -1.0, None, ALU.add, None, r=[d8b], w=[d8b])
            cp("dve", d8i, d8, r=[d8b], w=[d8ib])
            fill, fillb = AR.alloc(768, I32_, "fill")
            S.op("pool", lambda e: e.memset(fill, NTOK), w=[fillb])
            slotB = Buf("slot_tok")
            S.dma("sp", lambda e: e.dma_start(out=SLOT_TOK.rearrange("(p c) o -> p (c o)", p=128), in_=fill), r=[fillb], w=[slotB])
            for i in range(32):
                for j in range(8):
                    def _scat(e, i=i, j=j):
                        try:
                            return e.indirect_dma_start(
                                out=SLOT_TOK[:, :], out_offset=bass.IndirectOffsetOnAxis(ap=d8i3[:, i, j:j + 1], axis=0),
                                in_=tok_t[:, 2 * i:2 * i + 2], in_offset=None)
                        except Exception:
                            print("SCATTER FAIL at", i, j, d8i3[:, i, j:j + 1].shape, tok_t[:, 2 * i:2 * i + 2].shape, flush=True)
                            raise
                    S.dma("pool", _scat, r=[d8ib, tokb], wm=[slotB])
            cmp3, cmpb = AR.alloc(192 * 64, F32, "cmp3"); cmp33 = cmp3.rearrange("p (b e) -> p b e", b=192)
            tt("dve", cmp33, pend[:, None, :].to_broadcast([128, 192, 64]), bst.unsqueeze(2).to_broadcast([128, 192, 64]), ALU.is_le,
               r=[pendb, bstb], w=[cmpb])
            be_, beb = AR.alloc(192, F32, "be")
            S.op("dve", lambda e: e.tensor_reduce(out=be_, in_=cmp33, axis=AX.X, op=ALU.add), r=[cmpb], w=[beb])
            ts("dve", be_, be_, 63.0, 128.0, ALU.min, ALU.mult, r=[beb], w=[beb])
            ts("dve", be_, be_, pc[:, 0:1], None, ALU.add, None, r=[beb, bstb], w=[beb])
            widx, widxb = AR.alloc(192, I32_, "widx")
            cp("dve", widx, be_, r=[beb], w=[widxb])
            idb, idbb = AR.alloc(128, BF16, "identb")
            ld(idb, identb_d, w=[idbb])
            ixall, ixallb = AR.alloc(NBLK * 4, I32_, "ixall")
            for b in range(NBLK):
                for hf in range(2):
                    S.dma("sp", lambda e, b=b, hf=hf: e.dma_start(out=ixall[:, b * 4 + 2 * hf: b * 4 + 2 * hf + 2],
                          in_=SLOT_TOK[b * 256 + hf * 128: b * 256 + (hf + 1) * 128, :]), r=[slotB], wm=[ixallb])
            ixtoks = []
            wsl = Rot([(AR.alloc(2048, BF16, "wg%d" % i), AR.alloc(2048, BF16, "wu%d" % i), AR.alloc(2048, BF16, "wd%d" % i)) for i in range(4)])
            xgs = Rot([AR.alloc(2 * 1024, BF16, "xg%d" % i) for i in range(4)])
            xTs = Rot([AR.alloc(8 * 256, BF16, "xT%d" % i) for i in range(2)])
            sgs = Rot([AR.alloc(256, F32, "sg%d" % i) for i in range(2)])
            hids = Rot([AR.alloc(2 * 256, BF16, "hid%d" % i) for i in range(2)])
            yos = Rot([AR.alloc(1024, F32, "yo%d" % i) for i in range(3)])
            ptr = Rot(PS[0:2]); pgu = Rot([(PS[2], PS[3]), (PS[4], PS[5])]); pdn = Rot(PS[6:8])
            yslotB = Buf("yslot")
            blk = {}

            def blk_gather(b):
                ix, ixb = ixall[:, b * 4:(b + 1) * 4], ixallb
                (wg, wgb), (wu, wub), (wd, wdb) = wsl.next()
                for (wt_, wtb_, src_) in ((wg, wgb, EWG2), (wu, wub, EWU2), (wd, wdb, EWD2)):
                    S.dma("pool", lambda e, wt_=wt_, src_=src_, b=b: e.indirect_dma_start(
                        out=wt_, out_offset=None, in_=src_[:, :], in_offset=bass.IndirectOffsetOnAxis(ap=widx[:, b:b + 1], axis=0)),
                        r=[widxb], w=[wtb_])
                xg, xgb = xgs.next()
                for hf in range(2):
                    S.dma("pool", lambda e, xg=xg, ix=ix, hf=hf: e.indirect_dma_start(
                        out=xg[:, hf * 1024:(hf + 1) * 1024], out_offset=None, in_=H1B[:, :],
                        in_offset=bass.IndirectOffsetOnAxis(ap=ix[:, 2 * hf:2 * hf + 1], axis=0)),
                        r=[ixb], w=[xgb])
                blk[b] = {"w": (wg, wgb, wu, wub, wd, wdb), "xg": (xg, xgb)}

            def blk_trans(b):
                xg, xgb = blk[b]["xg"]
                xT, xTb = xTs.next()
                xT3 = xT.rearrange("p (k t) -> p k t", k=8)
                for hf in range(2):
                    for k4 in range(2):
                        p, pb = ptr.next()
                        pbv = p.bitcast(BF16)
                        for q in range(4):
                            k = k4 * 4 + q
                            tr(pbv[:, q * 128:(q + 1) * 128], xg[:, hf * 1024 + k * 128: hf * 1024 + (k + 1) * 128], idb, r=[xgb, idbb], w=[pb])
                        S.op("act", lambda e, pbv=pbv, k4=k4, hf=hf, xT3=xT3: e.activation(
                            out=xT3[:, k4 * 4:(k4 + 1) * 4, hf * 128:(hf + 1) * 128], in_=pbv[:, 0:512].rearrange("p (a b) -> p a b", a=4), func=AF.Copy),
                            r=[pb], w=[xTb])
                blk[b]["xT"] = (xT3, xTb)

            def blk_gu(b):
                wg, wgb, wu, wub, wd, wdb = blk[b]["w"]
                xT3, xTb = blk[b]["xT"]
                g3 = wg.rearrange("p (k f) -> p k f", k=8); u3 = wu.rearrange("p (k f) -> p k f", k=8)
                hd, hdb = hids.next()
                hd3 = hd.rearrange("p (f t) -> p f t", f=2)
                for fh in range(2):
                    (pG, pGb), (pU, pUb) = pgu.next()
                    for k in range(8):
                        mm(pG[:, 0:256], g3[:, k, fh * 128:(fh + 1) * 128], xT3[:, k, :], k == 0, k == 7, r=[wgb, xTb], w=[pGb])
                    for k in range(8):
                        mm(pU[:, 0:256], u3[:, k, fh * 128:(fh + 1) * 128], xT3[:, k, :], k == 0, k == 7, r=[wub, xTb], w=[pUb])
                    sg_, sgb = sgs.next()
                    act(sg_, pG[:, 0:256], AF.Silu, r=[pGb], w=[sgb])
                    tt("dve", hd3[:, fh, :], sg_, pU[:, 0:256], ALU.mult, r=[sgb, pUb], w=[hdb])
                blk[b]["hid"] = (hd3, hdb)

            def blk_down(b):
                wg, wgb, wu, wub, wd, wdb = blk[b]["w"]
                d3 = wd.rearrange("p (k d) -> p k d", k=2)
                hd3, hdb = blk[b]["hid"]
                for hf in range(2):
                    yo, yob = yos.next()
                    for dh in range(2):
                        p, pb = pdn.next()
                        for fk in range(2):
                            mm(p, hd3[:, fk, hf * 128:(hf + 1) * 128], d3[:, fk, dh * 512:(dh + 1) * 512], fk == 0, fk == 1, r=[hdb, wdb], w=[pb])
                        if dh == 0:
                            cp("act", yo[:, 0:512], p, r=[pb], w=[yob])
                        else:
                            cp("dve", yo[:, 512:1024], p, r=[pb], w=[yob])
                    S.dma("sp", lambda e, yo=yo, b=b, hf=hf: e.dma_start(out=YSLOT[b * 256 + hf * 128: b * 256 + (hf + 1) * 128, :], in_=yo),
                          r=[yob], wm=[yslotB])
                del blk[b]

            blk_gather(0)
            blk_gather(1)
            blk_gather(2)
            blk_trans(0)
            for b in range(NBLK):
                blk_gu(b)
                if b + 1 < NBLK:
                    blk_trans(b + 1)
                if b + 3 < NBLK:
                    blk_gather(b + 3)
                blk_down(b)
            moe["yslotB"] = yslotB

        def stage5_sparse_finish(s):
            AR.off = moe["keep"]
            rw83, rw8b = moe["rw8"]; d8i3, d8ib = moe["d8i"]; yslotB = moe["yslotB"]
            hT, hTb = AR.alloc(8 * L, BF16, "h1T"); hT3 = hT.rearrange("p (k t) -> p k t", k=8)
            ld(hT3, H1T[s].rearrange("(k p) t -> p k t", p=128), w=[hTb])
            acc, accb = AR.alloc(16 * 1024, F32, "acc"); acc3 = acc.rearrange("p (a d) -> p a d", a=16)
            accbs = [Buf("acc%d" % i) for i in range(16)]
            g_, gb_ = AR.alloc(8 * 256, BF16, "swg"); u_, _ = AR.alloc(8 * 256, BF16, "swu"); d_, _ = AR.alloc(2 * 1024, BF16, "swd")
            g3 = g_.rearrange("p (k f) -> p k f", k=8); u3 = u_.rearrange("p (k f) -> p k f", k=8); d3 = d_.rearrange("p (k d) -> p k d", k=2)
            if s == 0:
                S.dma("pool", lambda e: e.dma_start(out=g3, in_=ewg[64].rearrange("(k p) f -> p k f", p=128)), w=[gb_])
                S.dma("pool", lambda e: e.dma_start(out=u3, in_=ewu[64].rearrange("(k p) f -> p k f", p=128)), w=[gb_])
                S.dma("pool", lambda e: e.dma_start(out=d3, in_=ewd[64].rearrange("(k p) d -> p k d", p=128)), w=[gb_])
                moe["shw"] = gb_
            else:
                gb_ = moe["shw"]
            sg = Rot([AR.alloc(512, F32, "sg%d" % i) for i in range(2)])
            hid = Rot([AR.alloc(2 * 512, BF16, "hid%d" % i) for i in range(2)])
            pg = Rot([(PS[0], PS[1]), (PS[2], PS[3])]); pd = Rot(PS[4:8])
            for tb in range(4):
                hd, hdb = hid.next()
                hd3 = hd.rearrange("p (f t) -> p f t", f=2)
                for fh in range(2):
                    (pG, pGb), (pU, pUb) = pg.next()
                    for k in range(8):
                        mm(pG, g3[:, k, fh * 128:(fh + 1) * 128], hT3[:, k, tb * 512:(tb + 1) * 512], k == 0, k == 7, r=[gb_, hTb], w=[pGb])
                    for k in range(8):
                        mm(pU, u3[:, k, fh * 128:(fh + 1) * 128], hT3[:, k, tb * 512:(tb + 1) * 512], k == 0, k == 7, r=[gb_, hTb], w=[pUb])
                    sg_, sgb = sg.next()
                    act(sg_, pG, AF.Silu, r=[pGb], w=[sgb])
                    tt("dve", hd3[:, fh, :], sg_, pU, ALU.mult, r=[sgb, pUb], w=[hdb])
                for q in range(4):
                    t_ = tb * 4 + q
                    for dh in range(2):
                        p, pb = pd.next()
                        for fk in range(2):
                            mm(p, hd3[:, fk, q * 128:(q + 1) * 128], d3[:, fk, dh * 512:(dh + 1) * 512], fk == 0, fk == 1, r=[hdb, gb_], w=[pb])
                        cp("act", acc3[:, t_, dh * 512:(dh + 1) * 512], p, r=[pb], w=[accbs[t_]])
            ygs = Rot([AR.alloc(1024, F32, "yg%d" % i) for i in range(8)])
            for t_ in range(16):
                i = s * 16 + t_
                for j in range(8):
                    yg, ygb = ygs.next()
                    S.dma("pool", lambda e, yg=yg, i=i, j=j: e.indirect_dma_start(
                        out=yg, out_offset=None, in_=YSLOT[:, :], in_offset=bass.IndirectOffsetOnAxis(ap=d8i3[:, i, j:j + 1], axis=0)), r=[d8ib, yslotB], w=[ygb])
                    stt("dve", acc3[:, t_, :], yg, rw83[:, i, j:j + 1], acc3[:, t_, :], ALU.mult, ALU.add, r=[ygb, rw8b, accbs[t_]], w=[accbs[t_]])
            rows, rowsb = AR.alloc(2048, F32, "rows2")
            ld(rows[:, 0:1024], rowb("ln2g"), w=[rowsb]); ld(rows[:, 1024:2048], rowb("ln2b"), w=[rowsb])
            hr = Rot([AR.alloc(1024, F32, "hr%d" % i) for i in range(2)])
            oo = Rot([AR.alloc(1024, F32, "oo%d" % i) for i in range(2)])
            sq, sqb = AR.alloc(1024, F32, "sq2"); st8, st8b = AR.alloc(8, F32, "st8b")
            for t_ in range(16):
                h_, hb_ = hr.next()
                ld(h_, H1[s, t_ * 128:(t_ + 1) * 128, :], w=[hb_])
                stt("dve", h_, h_, ALPHA, acc3[:, t_, :], ALU.mult, ALU.add, r=[hb_, accbs[t_]], w=[hb_])
                o_, ob_ = oo.next()
                layer_norm_tile(h_, hb_, rows[:, 0:1024], rows[:, 1024:2048], rowsb, o_, ob_, st8, st8b, sq, sqb)
                stor(out[s, t_ * 128:(t_ + 1) * 128, :], o_, r=[ob_])

        nstage = dbg.get("_nstage", 99) if dbg else 99
        stage0()
        S.barrier()
        for s in range(NSEQ if nstage >= 1 else 0):
            stage1(s)
            S.barrier()
        for s in range(NSEQ if nstage >= 2 else 0):
            for cb in range(2):
                hyena_conv(s, cb, 0, HY[s, 2048 + cb * 512: 2048 + (cb + 1) * 512, :], HY[s, cb * 512:(cb + 1) * 512, :],
                           ZS[s, cb * 512:(cb + 1) * 512, :], F32)
                S.barrier()
                hyena_conv(s, cb, 1, ZS[s, cb * 512:(cb + 1) * 512, :], HY[s, 1024 + cb * 512: 1024 + (cb + 1) * 512, :],
                           YH[s, cb * 512:(cb + 1) * 512, :], BF16)
                S.barrier()
        for s in range(NSEQ if nstage >= 3 else 0):
            stage3(s)
            S.barrier()
        for s in range(NSEQ if nstage >= 4 else 0):
            stage4(s)
            S.barrier()
        if nstage >= 5:
            if SPARSE_MOE:
                stage5_sparse_blocks()
                S.barrier()
                for s in range(NSEQ):
                    stage5_sparse_finish(s)
                    S.barrier()
            else:
                for s in range(NSEQ):
                    stage5(s)
                    S.barrier()
        S.emit()
    return nc


_CONST = None


def _constants():
    global _CONST
    if _CONST is not None:
        return _CONST
    bf = ml_dtypes.bfloat16
    a = np.arange(L, dtype=np.int64)
    prod = np.outer(a, a) % 4096
    ang = prod.astype(np.float64) * (2.0 * np.pi / 4096.0)
    Cm = np.cos(ang).astype(np.float32)
    Sm = np.sin(ang).astype(np.float32)
    tile_ = lambda M: np.ascontiguousarray(M.reshape(16, 128, L).transpose(1, 0, 2)).astype(bf)
    c = {"dftc": tile_(Cm), "dfts": tile_(Sm)}
    t = np.linspace(0.0, 1.0, L, dtype=np.float32)[:, None]
    bands = 16
    w = (2.0 * np.pi * np.arange(L, dtype=np.float32)[:, None] / L).astype(np.float32)
    f = np.linspace(1e-4, bands - 1, bands, dtype=np.float32)[None, :]
    z = np.concatenate([t, np.cos(f * w), -np.sin(f * w)], axis=-1).astype(np.float32)
    c["zT"] = np.ascontiguousarray(z.T)
    max_decay = math.log(1e-2) / 0.3
    min_decay = math.log(1e-2) / 1.5
    deltas = np.linspace(min_decay, max_decay, 1024, dtype=np.float32)[None, :]
    c["decay"] = np.exp(-t * np.abs(deltas)).astype(np.float32)
    alt = np.where(np.arange(L) % 2 == 0, 1.0, -1.0).astype(np.float32)
    c["altcol"] = alt[:128, None].astype(bf)
    c["altrow"] = alt[None, :].astype(bf)
    wsc = np.full((128, 2), 2.0 / 4096.0, np.float32)
    wsc[0, 0] = 1.0 / 4096.0
    c["wsc"] = wsc
    c["identf"] = np.eye(128, dtype=np.float32)
    c["identb"] = np.eye(128, dtype=np.float32).astype(bf)
    s_ = np.arange(128)[:, None]; l_ = np.arange(128)[None, :]
    c["maskf"] = (l_ >= s_).astype(np.float32)
    c["maskb"] = (l_ <= s_).astype(np.float32)
    c["ones"] = np.ones((128, 128), np.float32)
    sel = np.zeros((32, 32, 128), np.float32)
    for h in range(32):
        sel[h, h, :] = 1.0
    c["selc"] = sel.reshape(32, 4096)
    c["tokid"] = np.repeat((np.arange(32)[None, :] * 128 + np.arange(128)[:, None]).astype(np.int32), 2, axis=1)
    c["bstart"] = (np.arange(192, dtype=np.float32) * 256.0)[None, :]
    c["pcol"] = np.arange(128, dtype=np.float32)[:, None]
    c["strictu"] = (l_ > s_).astype(np.float32)
    _CONST = c
    return c


def _pack_params(p):
    f32 = np.float32
    b_in = p["b_in"][0]
    col = np.zeros((128, NCOL), f32)
    hcw, hcb = p["hy_conv_w"][0], p["hy_conv_b"][0]
    for j in range(24):
        sl = slice(j * 128, (j + 1) * 128)
        col[:, CP_HY + 5 * j] = b_in[sl]
        for k in range(3):
            col[:, CP_HY + 5 * j + 1 + k] = hcw[k, sl]
        col[:, CP_HY + 5 * j + 4] = hcb[sl]
    scw, scb = p["ssd_conv_w"][0], p["ssd_conv_b"][0]
    for j in range(32):
        sl = slice(j * 128, (j + 1) * 128)
        col[:, CP_XBC + 7 * j] = b_in[5120 + j * 128: 5120 + (j + 1) * 128]
        for k in range(5):
            col[:, CP_XBC + 7 * j + 1 + k] = scw[k, sl]
        col[:, CP_XBC + 7 * j + 6] = scb[sl]
    for j in range(16):
        col[:, CP_GATE + j] = b_in[9280 + j * 128: 9280 + (j + 1) * 128]
    hb = p["hy_bias"][0]
    for o in range(2):
        for ct in range(8):
            col[:, CP_HYB + o * 8 + ct] = hb[o, ct * 128:(ct + 1) * 128]
    row = np.zeros((1, NROW), f32)

    def put(name, v):
        o, l = RP[name]
        row[0, o:o + l] = np.asarray(v, f32).reshape(-1)
    put("bz", b_in[3072:5120]); put("bdt", b_in[9216:9280]); put("dtb", p["ssd_dt_bias"][0]); put("alog", p["ssd_a_log"][0])
    put("dskip", np.repeat(p["ssd_d"][0], 64)); put("normw", p["ssd_norm_w"][0]); put("bout", p["b_out"][0])
    put("ln1g", p["ln1_g"][0]); put("ln1b", p["ln1_b"][0]); put("ln2g", p["ln2_g"][0]); put("ln2b", p["ln2_b"][0])
    put("rbias", p["router_bias"][0])
    fcols = np.stack([p["hy_f_b1"][0], p["hy_f_freq1"][0], p["hy_f_b2"][0], p["hy_f_freq2"][0], p["hy_f_b3"][0], p["hy_f_freq3"][0]], axis=1).astype(f32)
    d = {
        "w_in": np.ascontiguousarray(p["w_in"][0]), "colpack": col, "rowpack": row,
        "fw1": np.ascontiguousarray(p["hy_f_w1"][0]), "fw2": np.ascontiguousarray(p["hy_f_w2"][0]),
        "fw3": np.ascontiguousarray(p["hy_f_w3"][0]), "fw4": np.ascontiguousarray(p["hy_f_w4"][0]), "fcols": np.ascontiguousarray(fcols),
        "w_hyb": np.ascontiguousarray(p["w_hy_branch"][0]), "w_ssdb": np.ascontiguousarray(p["w_ssd_branch"][0]),
        "w_out": np.ascontiguousarray(p["w_out"][0]), "router_w": np.ascontiguousarray(p["router_w"][0]),
        "ewg": np.concatenate([p["exp_w_gate"][0], p["sh_w_gate"]], axis=0),
        "ewu": np.concatenate([p["exp_w_up"][0], p["sh_w_up"]], axis=0),
        "ewd": np.concatenate([p["exp_w_down"][0], p["sh_w_down"]], axis=0),
    }
    return d


_NC_CACHE = {}


def kernel(**inputs):
    p = {k: np.asarray(v) for k, v in inputs.items()}
    x = np.ascontiguousarray(p["x"], dtype=np.float32)
    shared = dict(_constants())
    shared.update(_pack_params(p))
    n = 8
    in_maps = []
    for c in range(n):
        xs = x[c * NSEQ:(c + 1) * NSEQ]
        m = dict(shared)
        m["x"] = np.ascontiguousarray(xs)
        m["xT"] = np.ascontiguousarray(xs.transpose(0, 2, 1))
        in_maps.append(m)
    if "nc" not in _NC_CACHE:
        _NC_CACHE["nc"] = build_program()
    nc = _NC_CACHE["nc"]
    res = run_bass_kernel_spmd(nc, in_maps, core_ids=list(range(n)))
    outs = [np.asarray(r["out"]) for r in res.results]
    return np.concatenate(outs, axis=0).astype(np.float32)
```
